# Optimizing a Trainium2 kernel written in Bass

```python
import jax
import jax.numpy as jnp
from jax import lax
import numpy as np

D_MODEL = 1024
BATCH = 8
SEQ = 2048
DEPTH = 2

GRID_W = 64
CTX_LEN = 256
N_EVEN = (DEPTH + 1) // 2
N_ODD = DEPTH // 2

A_WIDTH = 512
A_HEADS = 8
A_HEAD_DIM = A_WIDTH // A_HEADS
A_CONV_W = 4
LRU_C = 8.0
B_WIDTH = 512
B_CONV_W = 31
AB_IN = 2 * A_WIDTH + 2 * B_WIDTH
AB_OUT = A_WIDTH + B_WIDTH

C_WIDTH = 512
C_HEADS = 8
C_HEAD_DIM = C_WIDTH // C_HEADS
CHUNK = 128
D_WIDTH = 512
D_GROUPS = 4
D_GROUP_DIM = D_WIDTH // D_GROUPS
CD_IN = 2 * C_WIDTH + D_WIDTH
CD_OUT = C_WIDTH + D_WIDTH

D_FF = 2816
N_EXPERTS = 8
TOP_K = 2
D_FF_EXPERT = 3584

NORM_EPS = 1e-6
LN_EPS = 1e-5
POS_THETA = 10000.0

kernel_name = "hybrid_rglru_conformer_gmlp_fnet_moe_prefix_dit"


def rmsnorm(x, g):
    xf = x.astype(jnp.float32)
    xf = xf * lax.rsqrt(jnp.mean(xf * xf, axis=-1, keepdims=True) + NORM_EPS)
    return (xf * g.astype(jnp.float32)).astype(x.dtype)


def layernorm(x, g, b):
    xf = x.astype(jnp.float32)
    mu = jnp.mean(xf, axis=-1, keepdims=True)
    var = jnp.mean(jnp.square(xf - mu), axis=-1, keepdims=True)
    y = (xf - mu) * lax.rsqrt(var + LN_EPS) * g.astype(jnp.float32) + b.astype(jnp.float32)
    return y.astype(x.dtype)


def dwconv(x, w, b):
    y = lax.conv_general_dilated(x, w[:, None, :], window_strides=(1,), padding='SAME',
                                 dimension_numbers=('NWC', 'WIO', 'NWC'),
                                 feature_group_count=x.shape[-1])
    return y + b


def grid_pos_embed(rows):
    row = jnp.repeat(jnp.arange(rows, dtype=jnp.float32), GRID_W)
    col = jnp.tile(jnp.arange(GRID_W, dtype=jnp.float32), rows)
    n_freq = D_MODEL // 4
    omega = POS_THETA ** (-jnp.arange(n_freq, dtype=jnp.float32) / n_freq)
    ang_r = row[:, None] * omega
    ang_c = col[:, None] * omega
    return jnp.concatenate([jnp.sin(ang_r), jnp.cos(ang_r), jnp.sin(ang_c), jnp.cos(ang_c)], axis=-1)


def ada_terms(cvec, w_ada, b_ada):
    m = jax.nn.silu(cvec) @ w_ada + b_ada
    if m.ndim == 2:
        m = m[:, None, :]
    return jnp.split(m, 6, axis=-1)


def lru_coeffs(x, w_r, b_r, w_i, b_i, lam):
    bsz, length, _ = x.shape
    xh = x.reshape(bsz, length, A_HEADS, A_HEAD_DIM)
    r = jax.nn.sigmoid(jnp.einsum('blhd,hde->blhe', xh, w_r).reshape(bsz, length, A_WIDTH).astype(jnp.float32)
                       + b_r.astype(jnp.float32))
    i = jax.nn.sigmoid(jnp.einsum('blhd,hde->blhe', xh, w_i).reshape(bsz, length, A_WIDTH).astype(jnp.float32)
                       + b_i.astype(jnp.float32))
    log_a = -LRU_C * r * jax.nn.softplus(-lam.astype(jnp.float32))
    b = jnp.sqrt(-jnp.expm1(2.0 * log_a)) * (i * x.astype(jnp.float32))
    return jnp.exp(log_a), b


def linear_scan(a, b, h0):
    def combine(left, right):
        return left[0] * right[0], right[0] * left[1] + right[1]
    a_cum, b_cum = lax.associative_scan(combine, (a, b), axis=1)
    return a_cum * h0[:, None, :] + b_cum


def rglru_direction(x_ctx, x_lat, w_r, b_r, w_i, b_i, lam, reverse):
    a_c, b_c = lru_coeffs(x_ctx, w_r, b_r, w_i, b_i, lam)
    a_l, b_l = lru_coeffs(x_lat, w_r, b_r, w_i, b_i, lam)
    if reverse:
        a_c, b_c, a_l, b_l = (jnp.flip(t, axis=1) for t in (a_c, b_c, a_l, b_l))
    h_c = linear_scan(a_c, b_c, jnp.zeros_like(b_c[:, 0]))
    h_l = linear_scan(a_l, b_l, h_c[:, -1])
    if reverse:
        h_c, h_l = jnp.flip(h_c, axis=1), jnp.flip(h_l, axis=1)
    return h_c, h_l


def mixer_ab(h_lat, h_ctx, w_in, conv_w, conv_b, w_r, b_r, w_i, b_i, lam, dw_w, dw_b, ln_g, ln_b, w_out):
    def branches(h):
        z = h @ w_in
        xa, ga, vb, gb = jnp.split(z, [A_WIDTH, 2 * A_WIDTH, 2 * A_WIDTH + B_WIDTH], axis=-1)
        xa = dwconv(xa, conv_w, conv_b)
        u = dwconv(vb * jax.nn.sigmoid(gb), dw_w, dw_b)
        u = jax.nn.silu(layernorm(u, ln_g, ln_b))
        return xa, jax.nn.gelu(ga), u
    xa_c, ga_c, u_c = branches(h_ctx)
    xa_l, ga_l, u_l = branches(h_lat)
    fwd_c, fwd_l = rglru_direction(xa_c, xa_l, w_r[0], b_r[0], w_i[0], b_i[0], lam[0], reverse=False)
    bwd_c, bwd_l = rglru_direction(xa_c, xa_l, w_r[1], b_r[1], w_i[1], b_i[1], lam[1], reverse=True)
    rec_c = (fwd_c + bwd_c).astype(ga_c.dtype) * ga_c
    rec_l = (fwd_l + bwd_l).astype(ga_l.dtype) * ga_l
    y_c = jnp.concatenate([rec_c, u_c], axis=-1) @ w_out
    y_l = jnp.concatenate([rec_l, u_l], axis=-1) @ w_out
    return y_l, y_c


def spatial_gating(u, v, ln_g, ln_b, w_s, b_s):
    bsz, length, _ = v.shape
    v = layernorm(v, ln_g, ln_b)
    vh = v.reshape(bsz, length // CHUNK, CHUNK, C_HEADS, C_HEAD_DIM)
    mixed = jnp.einsum('hpq,bnqhd->bnphd', w_s, vh) + b_s.T[:, :, None]
    return u * mixed.reshape(bsz, length, C_WIDTH)


def fourier_mix(z):
    bsz, length, _ = z.shape
    zg = z.astype(jnp.float32).reshape(bsz, length, D_GROUPS, D_GROUP_DIM).transpose(0, 2, 1, 3)
    f = jnp.fft.fft2(zg, norm='ortho').real
    return f.transpose(0, 2, 1, 3).reshape(bsz, length, D_WIDTH).astype(z.dtype)


def mixer_cd(h, w_in, ln_g, ln_b, w_s, b_s, w_out):
    z = h @ w_in
    uv, f = z[..., :2 * C_WIDTH], z[..., 2 * C_WIDTH:]
    u, v = jnp.split(jax.nn.gelu(uv), 2, axis=-1)
    return jnp.concatenate([spatial_gating(u, v, ln_g, ln_b, w_s, b_s), fourier_mix(f)], axis=-1) @ w_out


def swiglu(x, w1, w3, w2):
    return (jax.nn.silu(x @ w1) * (x @ w3)) @ w2


def moe_swiglu(x, router, w1, w3, w2):
    logits = (x @ router).astype(jnp.float32)
    top_v, top_i = lax.top_k(logits, TOP_K)
    probs = jax.nn.softmax(top_v, axis=-1)
    gates = jnp.sum(jax.nn.one_hot(top_i, N_EXPERTS, dtype=jnp.float32) * probs[..., None], axis=-2)
    gates = gates.astype(x.dtype)
    out = jnp.zeros_like(x)
    for e in range(N_EXPERTS):
        out = out + gates[..., e:e + 1] * swiglu(x, w1[e], w3[e], w2[e])
    return out


def setup_inputs(seed: int = 0) -> dict:
    key = jax.random.key(seed)
    ks = iter(jax.random.split(key, 48))
    D = D_MODEL

    def nrm(shape, scale):
        return scale * jax.random.normal(next(ks), shape, jnp.float32)

    u = jax.random.uniform(next(ks), (N_EVEN, 2, A_WIDTH), jnp.float32, 0.9, 0.999)
    a0 = u ** (1.0 / LRU_C)
    rg_lambda = jnp.log(a0) - jnp.log1p(-a0)
    return {
        'x': nrm((BATCH, SEQ, D), 1.0),
        'c': nrm((BATCH, D), 1.0),
        'ctx': nrm((BATCH, CTX_LEN, D), 1.0),
        'c_ctx': nrm((D,), 1.0),
        'ada_w': nrm((DEPTH, D, 6 * D), 0.5 * D ** -0.5),
        'ada_b': nrm((DEPTH, 6 * D), 0.02),
        'norm_g': 1.0 + nrm((DEPTH, 4, D), 0.1),
        'ab_w_in': nrm((N_EVEN, D, AB_IN), D ** -0.5),
        'rg_conv_w': nrm((N_EVEN, A_CONV_W, A_WIDTH), A_CONV_W ** -0.5),
        'rg_conv_b': nrm((N_EVEN, A_WIDTH), 0.02),
        'rg_w_r': nrm((N_EVEN, 2, A_HEADS, A_HEAD_DIM, A_HEAD_DIM), A_HEAD_DIM ** -0.5),
        'rg_b_r': nrm((N_EVEN, 2, A_WIDTH), 0.02),
        'rg_w_i': nrm((N_EVEN, 2, A_HEADS, A_HEAD_DIM, A_HEAD_DIM), A_HEAD_DIM ** -0.5),
        'rg_b_i': nrm((N_EVEN, 2, A_WIDTH), 0.02),
        'rg_lambda': rg_lambda,
        'cv_dw_w': nrm((N_EVEN, B_CONV_W, B_WIDTH), B_CONV_W ** -0.5),
        'cv_dw_b': nrm((N_EVEN, B_WIDTH), 0.02),
        'cv_ln_g': 1.0 + nrm((N_EVEN, B_WIDTH), 0.1),
        'cv_ln_b': nrm((N_EVEN, B_WIDTH), 0.02),
        'ab_w_out': nrm((N_EVEN, AB_OUT, D), AB_OUT ** -0.5),
        'ffn_w1': nrm((N_EVEN, D, D_FF), D ** -0.5),
        'ffn_w3': nrm((N_EVEN, D, D_FF), D ** -0.5),
        'ffn_w2': nrm((N_EVEN, D_FF, D), D_FF ** -0.5),
        'cd_w_in': nrm((N_ODD, D, CD_IN), D ** -0.5),
        'sg_ln_g': 1.0 + nrm((N_ODD, C_WIDTH), 0.1),
        'sg_ln_b': nrm((N_ODD, C_WIDTH), 0.02),
        'sg_w_s': nrm((N_ODD, C_HEADS, CHUNK, CHUNK), CHUNK ** -0.5),
        'sg_b_s': 1.0 + nrm((N_ODD, C_HEADS, CHUNK), 0.1),
        'cd_w_out': nrm((N_ODD, CD_OUT, D), CD_OUT ** -0.5),
        'moe_router': nrm((N_ODD, D, N_EXPERTS), D ** -0.5),
        'moe_w1': nrm((N_ODD, N_EXPERTS, D, D_FF_EXPERT), D ** -0.5),
        'moe_w3': nrm((N_ODD, N_EXPERTS, D, D_FF_EXPERT), D ** -0.5),
        'moe_w2': nrm((N_ODD, N_EXPERTS, D_FF_EXPERT, D), D_FF_EXPERT ** -0.5),
    }


def reference(x, c, ctx, c_ctx, ada_w, ada_b, norm_g,
              ab_w_in, rg_conv_w, rg_conv_b, rg_w_r, rg_b_r, rg_w_i, rg_b_i, rg_lambda,
              cv_dw_w, cv_dw_b, cv_ln_g, cv_ln_b, ab_w_out,
              ffn_w1, ffn_w3, ffn_w2,
              cd_w_in, sg_ln_g, sg_ln_b, sg_w_s, sg_b_s, cd_w_out,
              moe_router, moe_w1, moe_w3, moe_w2):
    ROWS = x.shape[1] // GRID_W
    x = x + grid_pos_embed(ROWS).astype(x.dtype)
    xc = ctx
    for layer in range(DEPTH):
        j = layer // 2
        keep_ctx = layer < DEPTH - 1
        sh1, sc1, g1, sh2, sc2, g2 = ada_terms(c, ada_w[layer], ada_b[layer])
        sh1c, sc1c, g1c, sh2c, sc2c, g2c = ada_terms(c_ctx, ada_w[layer], ada_b[layer])
        g_pre_mix, g_post_mix, g_pre_ffn, g_post_ffn = norm_g[layer]

        h = rmsnorm(x, g_pre_mix) * (1.0 + sc1) + sh1
        hc = rmsnorm(xc, g_pre_mix) * (1.0 + sc1c) + sh1c
        if layer % 2 == 0:
            y, yc = mixer_ab(h, hc, ab_w_in[j], rg_conv_w[j], rg_conv_b[j], rg_w_r[j], rg_b_r[j],
                             rg_w_i[j], rg_b_i[j], rg_lambda[j], cv_dw_w[j], cv_dw_b[j],
                             cv_ln_g[j], cv_ln_b[j], ab_w_out[j])
        else:
            y = mixer_cd(h, cd_w_in[j], sg_ln_g[j], sg_ln_b[j], sg_w_s[j], sg_b_s[j], cd_w_out[j])
            if keep_ctx:
                yc = mixer_cd(hc, cd_w_in[j], sg_ln_g[j], sg_ln_b[j], sg_w_s[j], sg_b_s[j], cd_w_out[j])
        x = x + g1 * rmsnorm(y, g_post_mix)
        if keep_ctx:
            xc = xc + g1c * rmsnorm(yc, g_post_mix)

        if layer % 2 == 0:
            ffn = lambda t: swiglu(t, ffn_w1[j], ffn_w3[j], ffn_w2[j])
        else:
            ffn = lambda t: moe_swiglu(t, moe_router[j], moe_w1[j], moe_w3[j], moe_w2[j])
        x = x + g2 * rmsnorm(ffn(rmsnorm(x, g_pre_ffn) * (1.0 + sc2) + sh2), g_post_ffn)
        if keep_ctx:
            xc = xc + g2c * rmsnorm(ffn(rmsnorm(xc, g_pre_ffn) * (1.0 + sc2c) + sh2c), g_post_ffn)
    return x
```

```python
import contextlib
import numpy as np
import concourse.bass as bass
import concourse.mybir as mybir
from concourse.bass_utils import run_bass_kernel_spmd

F32 = mybir.dt.float32
BF16 = mybir.dt.bfloat16
AF = mybir.ActivationFunctionType
ALU = mybir.AluOpType

D = 1024
T = 2048
CTX = 256
NT = T // 128
NR = T // 512
KC = D // 128
DFF = 2816
DFFE = 3584
NE = 8
FG = 256
NORM_EPS = 1e-6
LN_EPS = 1e-5
ENGS = ("pe", "act", "dve", "pool", "sp")


class _Op:
    __slots__ = ("eng", "fn", "reads", "writes", "dma", "deps", "sig", "needs_sig", "idx")

    def __init__(self, eng, fn, reads, writes, dma):
        self.eng, self.fn, self.reads, self.writes, self.dma = eng, fn, reads, writes, dma
        self.deps = []
        self.sig = None
        self.needs_sig = False


class Prog:
    DMA_POOL = 8

    def __init__(self, nc):
        self.nc = nc
        self.ops = []

    def op(self, eng, fn, reads=(), writes=(), dma=False, phase=True):
        rd = tuple(reads) + (("PHASE",) if phase else ())
        o = _Op(eng, fn, rd, tuple(writes), dma)
        o.needs_sig = dma
        o.idx = len(self.ops)
        self.ops.append(o)
        return o

    def _analyse(self):
        last_w, readers = {}, {}
        dma_hist = {e: [] for e in ENGS}
        ops = self.ops
        for o in ops:
            deps = set()
            for r in o.reads:
                w = last_w.get(r)
                if w is not None:
                    deps.add(w)
            for w in o.writes:
                lw = last_w.get(w)
                if lw is not None:
                    deps.add(lw)
                rl = readers.get(w)
                if rl:
                    deps.update(rl)
            if o.dma:
                h = dma_hist[o.eng]
                if len(h) >= self.DMA_POOL:
                    deps.add(h[-self.DMA_POOL])
                h.append(o.idx)
            deps.discard(o.idx)
            if o.eng == "pe":
                deps = {d for d in deps if ops[d].eng != "pe"}
            o.deps = deps
            for d in deps:
                ops[d].needs_sig = True
            for r in o.reads:
                readers.setdefault(r, []).append(o.idx)
            for w in o.writes:
                last_w[w] = o.idx
                readers[w] = []

    def emit(self):
        nc = self.nc
        self._analyse()
        stack = contextlib.ExitStack()
        csem = {e: stack.enter_context(nc.semaphore("c_" + e)) for e in ("pe", "act", "dve", "pool")}
        dsem = {e: [stack.enter_context(nc.semaphore("d_%s_%d" % (e, j))) for j in range(self.DMA_POOL)]
                for e in ("sp", "pool", "act")}
        ccount = {e: 0 for e in csem}
        dcount = {e: 0 for e in dsem}
        for o in self.ops:
            if not o.needs_sig:
                continue
            if o.dma:
                n = dcount[o.eng]
                dcount[o.eng] += 1
                o.sig = (dsem[o.eng][n % self.DMA_POOL], 16 * (n // self.DMA_POOL + 1), 16)
            else:
                ccount[o.eng] += 1
                o.sig = (csem[o.eng], ccount[o.eng], 1)
        per_eng = {e: [o for o in self.ops if o.eng == e] for e in ENGS}
        ops = self.ops
        K = self.DMA_POOL

        def run(engname, engobj):
            seen = {}
            for o in per_eng[engname]:
                need = {}
                for d in o.deps:
                    sem, val, _ = ops[d].sig
                    k = id(sem)
                    if seen.get(k, 0) >= val:
                        continue
                    if k not in need or need[k][1] < val:
                        need[k] = (sem, val)
                for k, (sem, val) in need.items():
                    engobj.wait_ge(sem, val)
                    seen[k] = val
                ins = o.fn(engobj)
                if o.needs_sig:
                    ins.then_inc(o.sig[0], o.sig[2])
            if engname == "sp":
                for e in dsem:
                    n = dcount[e]
                    for j in range(min(n, K)):
                        engobj.wait_ge(dsem[e][j], 16 * ((n - 1 - j) // K + 1))

        with nc.Block() as block:
            @block.tensor
            def _(e):
                run("pe", e)

            @block.scalar
            def _(e):
                run("act", e)

            @block.vector
            def _(e):
                run("dve", e)

            @block.gpsimd
            def _(e):
                run("pool", e)

            @block.sync
            def _(e):
                run("sp", e)
        stack.close()


class Region:
    def __init__(self, tensor, nwords):
        self.t = tensor
        self.n = nwords
        self.off = 0

    def reset(self):
        self.off = 0

    def mark(self):
        return self.off

    def release(self, m):
        self.off = m

    def alloc(self, shape, dt=F32):
        assert shape[0] == 128
        nel = int(np.prod(shape[1:]))
        words = nel if dt == F32 else (nel + 1) // 2
        assert self.off + words <= self.n, "region overflow: need %d words at %d of %d" % (words, self.off, self.n)
        v = self.t[:, self.off:self.off + words]
        self.off += words
        if dt != F32:
            v = v.bitcast(dt)[:, 0:nel]
        if len(shape) == 3:
            v = v.rearrange("p (a b) -> p a b", b=shape[2])
        elif len(shape) == 4:
            v = v.rearrange("p (a b c) -> p a b c", b=shape[2], c=shape[3])
        return v


def build(nc, layers=(0, 1), parts=("mix", "ffn"), moe_experts=NE, dbg=None):
    st = contextlib.ExitStack()

    def din(name, shape, dt=F32):
        return nc.dram_tensor(name, list(shape), dt, kind="ExternalInput").ap()

    x_d = din("x", [T, D])
    ctx_d = din("ctx", [CTX, D])
    cc_d = din("cc", [16, 128])
    ident_d = din("ident", [128, 128])
    ada_w = din("ada_w", [2, D, 6 * D])
    ada_b = din("ada_b", [2, 6 * D])
    norm_g = din("norm_g", [2, 4, D])
    ffn_w1 = din("ffn_w1", [D, DFF])
    ffn_w3 = din("ffn_w3", [D, DFF])
    ffn_w2 = din("ffn_w2", [DFF, D])
    moe_router = din("moe_router", [D, NE])
    moe_w1 = din("moe_w1", [NE, D, DFFE])
    moe_w3 = din("moe_w3", [NE, D, DFFE])
    moe_w2 = din("moe_w2", [NE, DFFE, D])
    ab_w_in = din("ab_w_in", [D, 2048])
    rg_conv_w = din("rg_conv_w", [4, 512])
    rg_conv_b = din("rg_conv_b", [1, 512])
    rg_w_r = din("rg_w_r", [2, 8, 64, 64])
    rg_b_r = din("rg_b_r", [2, 512])
    rg_w_i = din("rg_w_i", [2, 8, 64, 64])
    rg_b_i = din("rg_b_i", [2, 512])
    rg_lambda = din("rg_lambda", [2, 512])
    cv_dw_w = din("cv_dw_w", [31, 512])
    cv_dw_b = din("cv_dw_b", [1, 512])
    cv_ln_g = din("cv_ln_g", [1, 512])
    cv_ln_b = din("cv_ln_b", [1, 512])
    ab_w_out = din("ab_w_out", [D, D])
    cd_w_in = din("cd_w_in", [D, 1536])
    sg_ln_g = din("sg_ln_g", [1, 512])
    sg_ln_b = din("sg_ln_b", [1, 512])
    sg_w_s = din("sg_w_s", [8, 128, 128])
    sg_b_s = din("sg_b_s", [8, 128])
    cd_w_out = din("cd_w_out", [D, D])
    cs128_d = din("cs128", [128, 256], BF16)
    dft_d = din("dft", [NR, NT, 128, 2, 512], BF16)
    out_d = nc.dram_tensor("out", [T, D], F32, kind="ExternalOutput").ap()

    def sb(name, shape, dt=F32):
        return st.enter_context(nc.sbuf_tensor("sb_" + name, list(shape), dt))

    x_tm = sb("x_tm_sb", [128, NT, D])
    h_cm = sb("h_cm_sb", [128, KC, T], BF16)
    cols = sb("cols", [128, 384])
    ident = sb("ident_sb", [128, 128])
    gg_all = sb("gg_bc", [128, 2, 2, D], BF16)
    modc_all = sb("modc", [128, 2, 8, KC])
    ggtmp_holder = {}
    gates = sb("gates", [128, NT, NE])
    stat = sb("stat", [128, 64])
    dummy = sb("dummy", [128, 2])
    REG_WORDS = 25600
    reg_t = sb("region", [128, REG_WORDS])
    R = Region(reg_t, REG_WORDS)
    banks = [st.enter_context(nc.psum_tensor("bank%d" % i, [128, 512], F32)) for i in range(8)]

    P = Prog(nc)

    def barrier():
        P.op("pool", lambda e: e.memset(dummy[:, 0:1], 0.0), writes=["PHASE"], phase=False)
        R.reset()

    colmap = {}
    rows_used = [0]

    def phase0():
        rows = R.alloc([128, 3, 128])
        io_ = R.alloc([128, 256])
        om_ = R.alloc([128, 256])
        pv_ = R.alloc([128, 4])
        ang_ = R.alloc([128, 2, 256])
        tt_ = R.alloc([128, 2, 256])
        tab_ = R.alloc([128, 2, 512])
        s2_ = R.alloc([128, 2, 256])
        sel_ = R.alloc([128, 16, 128])[0:32]
        zer_ = R.alloc([128, 16, 128])[0:32]
        MAGIC = 12582912.0
        TWO_PI = 6.283185307179586
        P.op("pool", lambda e: e.iota(io_[:, :], [[1, 256]], base=0, channel_multiplier=0, allow_small_or_imprecise_dtypes=True), writes=["pe_io"])
        P.op("pool", lambda e: e.iota(pv_[0:32, 0:1], [[0, 1]], base=0, channel_multiplier=1, allow_small_or_imprecise_dtypes=True), writes=["pe_pv0"])
        P.op("pool", lambda e: e.iota(pv_[0:64, 1:2], [[0, 1]], base=0, channel_multiplier=1, allow_small_or_imprecise_dtypes=True), writes=["pe_pv1a"])
        P.op("pool", lambda e: e.iota(pv_[64:128, 1:2], [[0, 1]], base=0, channel_multiplier=1, allow_small_or_imprecise_dtypes=True), writes=["pe_pv1b"])
        P.op("pool", lambda e: e.iota(sel_.rearrange("k j (h q) -> k j h q", h=2), [[2, 16], [1, 2], [0, 64]], base=0, channel_multiplier=-1,
                                      allow_small_or_imprecise_dtypes=True), writes=["pe_sel0"])
        P.op("dve", lambda e: e.memset(zer_, 0.0), writes=["pe_zer"])
        P.op("dve", lambda e: e.tensor_tensor(out=sel_, in0=sel_, in1=zer_, op=ALU.is_equal), reads=["pe_sel0", "pe_zer"], writes=["pe_sel"])
        P.op("act", lambda e: e.activation(out=om_[:, :], in_=io_[:, :], func=AF.Exp, scale=-9.210340371976184 / 256.0), reads=["pe_io"], writes=["pe_om"])
        P.op("dve", lambda e: e.tensor_scalar(out=ang_[0:32, 0, :], in0=om_[0:32, :], scalar1=pv_[0:32, 0:1], scalar2=None, op0=ALU.mult),
             reads=["pe_om", "pe_pv0"], writes=["pe_ang0"])
        P.op("dve", lambda e: e.tensor_scalar(out=ang_[:, 1, :], in0=om_[:, :], scalar1=pv_[:, 1:2], scalar2=None, op0=ALU.mult),
             reads=["pe_om", "pe_pv1a", "pe_pv1b"], writes=["pe_ang1"])
        for i_, np_ in ((0, 32), (1, 128)):
            a_ = ang_[0:np_, i_, :]
            t_ = tt_[0:np_, i_, :]
            q_ = s2_[0:np_, i_, :]
            P.op("dve", lambda e, a_=a_, t_=t_: e.tensor_scalar(out=t_, in0=a_, scalar1=1.0 / TWO_PI, scalar2=MAGIC, op0=ALU.mult, op1=ALU.add),
                 reads=["pe_ang%d" % i_], writes=["pe_t%d" % i_])
            P.op("dve", lambda e, t_=t_: e.tensor_scalar(out=t_, in0=t_, scalar1=MAGIC, scalar2=-TWO_PI, op0=ALU.subtract, op1=ALU.mult), writes=["pe_t%d" % i_])
            P.op("dve", lambda e, a_=a_, t_=t_: e.tensor_tensor(out=t_, in0=t_, in1=a_, op=ALU.add), reads=["pe_ang%d" % i_], writes=["pe_t%d" % i_])
            P.op("dve", lambda e, t_=t_: e.tensor_scalar(out=t_, in0=t_, scalar1=-3.1415925, scalar2=3.1415925, op0=ALU.max, op1=ALU.min), writes=["pe_t%d" % i_])
            P.op("act", lambda e, t_=t_, i_=i_, np_=np_: e.activation(out=tab_[0:np_, i_, 0:256], in_=t_, func=AF.Sin), reads=["pe_t%d" % i_], writes=["pe_sin%d" % i_])
            P.op("act", lambda e, t_=t_, q_=q_: e.activation(out=q_, in_=t_, func=AF.Sin, scale=0.5), reads=["pe_t%d" % i_], writes=["pe_q%d" % i_])
            P.op("dve", lambda e, q_=q_: e.tensor_tensor(out=q_, in0=q_, in1=q_, op=ALU.mult), writes=["pe_q%d" % i_])
            P.op("dve", lambda e, q_=q_, i_=i_, np_=np_: e.tensor_scalar(out=tab_[0:np_, i_, 256:512], in0=q_, scalar1=-2.0, scalar2=1.0, op0=ALU.mult, op1=ALU.add),
                 reads=["pe_q%d" % i_], writes=["pe_cos%d" % i_])
        P.op("dve", lambda e: e.memset(rows[:, :, :], 0.0), writes=["rows"])
        P.op("sp", lambda e: e.dma_start(out=ident[:], in_=ident_d), writes=["ident"], dma=True)

        def load_rows(name, ap2d):
            n = ap2d.shape[0]
            r0 = rows_used[0]
            if (r0 % 128) + n > 128:
                r0 = (r0 // 128 + 1) * 128
            colmap[name] = r0
            rows_used[0] = r0 + n
            assert rows_used[0] <= 384
            P.op("sp", lambda e: e.dma_start(out=rows[r0 % 128:r0 % 128 + n, r0 // 128, :], in_=ap2d),
                 reads=["rows"], writes=[("rowsd", name)], dma=True)

        load_rows("cc", cc_d)
        for l in range(2):
            load_rows("normg%d" % l, norm_g[l].rearrange("a (c p) -> (a c) p", p=128))
            load_rows("adab%d" % l, ada_b[l:l + 1, :].rearrange("a (c p) -> (a c) p", p=128))
        extra_rows(load_rows)
        names = list(colmap.keys())
        for i in range(3):
            P.op("pe", lambda e, i=i: e.transpose(out=banks[0][:, i * 128:(i + 1) * 128], in_=rows[:, i, :], identity=ident[:]),
                 reads=[("rowsd", n) for n in names] + ["ident", "rows"], writes=[("b", 0)])
        P.op("dve", lambda e: e.tensor_copy(out=cols[:], in_=banks[0][:, 0:384]), writes=["cols", ("b", 0)])
        for j in range(NT):
            P.op("sp", lambda e, j=j: e.dma_start(out=x_tm[:, j, :], in_=x_d[j * 128:(j + 1) * 128, :]),
                 writes=[("x", j)], dma=True)
            pb_ = 1 + (j % 2)
            P.op("pe", lambda e, j=j, pb_=pb_: e.matmul(banks[pb_][:], lhsT=sel_[:, j, :], rhs=tab_[0:32, 0, :], start=True, stop=True),
                 reads=["pe_sel", "pe_sin0", "pe_cos0"], writes=[("b", pb_)])
            P.op("dve", lambda e, j=j, pb_=pb_: e.tensor_tensor(out=x_tm[:, j, 0:512], in0=x_tm[:, j, 0:512], in1=banks[pb_][:], op=ALU.add),
                 reads=[("x", j)], writes=[("x", j), ("b", pb_)])
            P.op("dve", lambda e, j=j: e.tensor_tensor(out=x_tm[:, j, 512:1024], in0=x_tm[:, j, 512:1024], in1=tab_[:, 1, :], op=ALU.add),
                 reads=[("x", j), "pe_sin1", "pe_cos1"], writes=[("x", j)])
        barrier()

    def extra_rows(load_rows):
        r2 = lambda ap: ap.rearrange("a (c p) -> (a c) p", p=128)
        load_rows("rg_conv_w", r2(rg_conv_w))
        load_rows("rg_conv_b", r2(rg_conv_b))
        load_rows("rg_b_r", r2(rg_b_r))
        load_rows("rg_b_i", r2(rg_b_i))
        load_rows("rg_lambda", r2(rg_lambda))
        load_rows("cv_dw_b", r2(cv_dw_b))
        load_rows("cv_ln_g", r2(cv_ln_g))
        load_rows("cv_ln_b", r2(cv_ln_b))
        load_rows("cv_dw_w", r2(cv_dw_w))

    def mod_steps(l, need_ctx, bb=1, do_barrier=True, nbuf=3, vlist=(0, 1, 2, 3, 4, 5)):
        gg_bc = gg_all[:, l]
        modc = modc_all[:, l]
        ggf = R.alloc([128, 2, 512])
        wa = [R.alloc([128, KC, 512], BF16) for _ in range(nbuf)]
        bc = R.alloc([128, 4, D])
        silu_cc = R.alloc([128, KC, 2], BF16)
        silu_rep = R.alloc([128, KC, 128], BF16)
        mraw = R.alloc([128, 4, KC, 2])
        c0 = colmap["cc"]
        P.op("act", lambda e: e.activation(out=silu_cc[:, :, 0], in_=cols[:, c0:c0 + 8], func=AF.Silu),
             reads=["cols"], writes=["silu0"])
        P.op("act", lambda e: e.activation(out=silu_cc[:, :, 1], in_=cols[:, c0 + 8:c0 + 16], func=AF.Silu),
             reads=["cols"], writes=["silu1"])
        for k in range(KC):
            P.op("dve", lambda e, k=k: e.tensor_copy(out=silu_rep[:, k, :], in_=silu_cc[:, k, 0:1].to_broadcast([128, 128])),
                 reads=["silu0"], writes=[("silurep", k)])
        srcs = [ada_b[l:l + 1, 2 * D:3 * D], ada_b[l:l + 1, 5 * D:6 * D], norm_g[l, 1:2, :], norm_g[l, 3:4, :]]
        for i, s in enumerate(srcs):
            P.op("sp", lambda e, i=i, s=s: e.dma_start(out=bc[:, i, :], in_=s.to_broadcast([128, D])),
                 writes=[("bc", i)], dma=True)
        nload = [0]
        ab0 = colmap["adab%d" % l]
        ng0 = colmap["normg%d" % l]
        def derive(dst, sci, shi, gcol, who):
            P.op("dve", lambda e: e.scalar_tensor_tensor(
                out=modc[:, dst, :], in0=mraw[:, sci, :, who], scalar=1.0, in1=cols[:, gcol:gcol + 8], op0=ALU.add, op1=ALU.mult),
                reads=[("mraw", sci), "cols"], writes=[("modc", l, dst)])
            P.op("dve", lambda e: e.tensor_copy(out=modc[:, dst + 1, :], in_=mraw[:, shi, :, who]),
                 reads=[("mraw", shi)], writes=[("modc", l, dst + 1)])

        for v in vlist:
            for half in range(2):
                n = nload[0]
                nload[0] += 1
                wb = wa[n % nbuf]
                src = ada_w[l][:, v * D + half * 512: v * D + (half + 1) * 512].rearrange("(k p) f -> p k f", p=128)
                P.op("pool", lambda e, wb=wb, src=src: e.dma_start(out=wb[:], in_=src), writes=[("wa", n % nbuf)], dma=True)
                if v in (2, 5):
                    gi = 0 if v == 2 else 1
                    bk = bb + (n % 2)

                    def mm(e, wb=wb, bk=bk):
                        for k in range(KC):
                            ins = e.matmul(banks[bk][:], lhsT=silu_rep[:, k, :], rhs=wb[:, k, :], start=(k == 0), stop=(k == KC - 1))
                        return ins
                    P.op("pe", mm, reads=[("wa", n % nbuf)] + [("silurep", k) for k in range(KC)], writes=[("b", bk)])
                    sl = slice(half * 512, (half + 1) * 512)
                    gt = ggf[:, n % 2, :]
                    P.op("dve", lambda e, bk=bk, gi=gi, sl=sl, gt=gt: e.tensor_tensor(out=gt, in0=banks[bk][:], in1=bc[:, gi, sl], op=ALU.add),
                         reads=[("bc", gi)], writes=[("ggf", n % 2), ("b", bk)])
                    P.op("dve", lambda e, gi=gi, sl=sl, gt=gt: e.tensor_tensor(out=gg_bc[:, gi, sl], in0=gt, in1=bc[:, 2 + gi, sl], op=ALU.mult),
                         reads=[("ggf", n % 2), ("bc", 2 + gi)], writes=[("gg", l, gi, half)])
                else:
                    vi = {0: 0, 1: 1, 3: 2, 4: 3}[v]

                    def mm(e, wb=wb, half=half):
                        for oc in range(4):
                            for k in range(KC):
                                ins = e.matmul(banks[bb + 2][:, (half * 4 + oc) * 2:(half * 4 + oc) * 2 + 2], lhsT=wb[:, k, oc * 128:(oc + 1) * 128],
                                               rhs=silu_cc[:, k, :], start=(k == 0), stop=(k == KC - 1))
                        return ins
                    P.op("pe", mm, reads=[("wa", n % nbuf), "silu0", "silu1"], writes=[("b", bb + 2)])
                    if half == 1:
                        a0 = ab0 + v * 8
                        P.op("dve", lambda e, vi=vi, a0=a0: e.tensor_tensor(
                            out=mraw[:, vi, :, :], in0=banks[bb + 2][:, 0:16].rearrange("p (a b) -> p a b", b=2),
                            in1=cols[:, a0:a0 + 8].unsqueeze(2).to_broadcast([128, 8, 2]), op=ALU.add),
                            reads=["cols"], writes=[("mraw", vi), ("b", bb + 2)])
                        if v == 1:
                            derive(0, 1, 0, ng0 + 0, 0)
                            if need_ctx:
                                derive(2, 1, 0, ng0 + 0, 1)
                        if v == 4:
                            derive(4, 3, 2, ng0 + 16, 0)
                yield
        if do_barrier:
            barrier()
        yield

    def phase_mod(l, need_ctx):
        for _ in mod_steps(l, need_ctx):
            pass

    def phase_norm(which, router=False, extra=None):
        sc_i, sh_i = (0, 1) if which == 0 else (4, 5)
        L = CL[0]
        modc = modc_all[:, L]
        junk = R.alloc([128, D], BF16)
        xs = R.alloc([128, 2, 4, D])
        ss = stat[:, 0:16]
        rstd = stat[:, 16:32]
        if router:
            h32 = R.alloc([128, 2, KC, 512])
            rt = R.alloc([128, KC, NE])
            lg = R.alloc([128, NT, NE])
            m8 = R.alloc([128, NT, 8])
            ex = R.alloc([128, NT, NE])
            P.op("sp", lambda e: e.dma_start(out=rt[:], in_=moe_router.rearrange("(k p) n -> p k n", p=128)), writes=["rt"], dma=True)
        P.op("dve", lambda e: e.memset(ss, 0.0), writes=["ss"])

        def stage0(r):
            for j in range(4 * r, 4 * r + 4):
                P.op("act", lambda e, j=j: e.activation(out=junk[:], in_=x_tm[:, j, :], func=AF.Square, accum_out=ss[:, j:j + 1]),
                     reads=[("x", j), "ss"], writes=[("ssj", j), "junk"])
            sl = slice(4 * r, 4 * r + 4)
            P.op("act", lambda e, sl=sl: e.activation(out=rstd[:, sl], in_=ss[:, sl], func=AF.Sqrt, bias=NORM_EPS, scale=1.0 / D),
                 reads=[("ssj", j) for j in range(4 * r, 4 * r + 4)], writes=[("rstd0", r)])
            P.op("dve", lambda e, sl=sl: e.reciprocal(out=rstd[:, sl], in_=rstd[:, sl]), reads=[("rstd0", r)], writes=[("rstd", r)])

        def stage1(r):
            for jj in range(4):
                j = 4 * r + jj
                P.op("dve", lambda e, j=j, jj=jj, r=r: e.tensor_scalar(out=xs[:, r % 2, jj, :], in0=x_tm[:, j, :], scalar1=rstd[:, j:j + 1], scalar2=None,
                                                                        op0=ALU.mult),
                     reads=[("x", j), ("rstd", r)], writes=[("xs", r % 2, jj)])

        def stage2(r):
            rb = r % 2
            for k in range(KC):
                u = r * KC + k
                bk = u % (5 if extra is not None else 6)

                def tr(e, rb=rb, k=k, bk=bk):
                    for jj in range(4):
                        ins = e.transpose(out=banks[bk][:, jj * 128:(jj + 1) * 128], in_=xs[:, rb, jj, k * 128:(k + 1) * 128], identity=ident[:])
                    return ins
                P.op("pe", tr, reads=[("xs", rb, jj) for jj in range(4)] + ["ident"], writes=[("b", bk)])
                hk = [("h", k, j) for j in range(4 * r, 4 * r + 4)]
                dst = h_cm[:, k, r * 512:(r + 1) * 512]
                if u % 2 == 0:
                    P.op("dve", lambda e, k=k, bk=bk, dst=dst: e.tensor_scalar(out=dst, in0=banks[bk][:], scalar1=modc[:, sc_i, k:k + 1],
                                                                                scalar2=modc[:, sh_i, k:k + 1], op0=ALU.mult, op1=ALU.add),
                         reads=[("modc", L, sc_i), ("modc", L, sh_i)], writes=hk + [("b", bk)])
                    if router:
                        P.op("dve", lambda e, k=k, bk=bk, rb=rb: e.tensor_scalar(out=h32[:, rb, k, :], in0=banks[bk][:], scalar1=modc[:, sc_i, k:k + 1],
                                                                                  scalar2=modc[:, sh_i, k:k + 1], op0=ALU.mult, op1=ALU.add),
                             reads=[("modc", L, sc_i), ("modc", L, sh_i)], writes=[("h32", rb, k), ("b", bk)])
                else:
                    P.op("act", lambda e, k=k, bk=bk, dst=dst: e.activation(out=dst, in_=banks[bk][:], func=AF.Identity, bias=modc[:, sh_i, k:k + 1],
                                                                             scale=modc[:, sc_i, k:k + 1]),
                         reads=[("modc", L, sc_i), ("modc", L, sh_i)], writes=hk + [("b", bk)])
                    if router:
                        P.op("act", lambda e, k=k, bk=bk, rb=rb: e.activation(out=h32[:, rb, k, :], in_=banks[bk][:], func=AF.Identity,
                                                                               bias=modc[:, sh_i, k:k + 1], scale=modc[:, sc_i, k:k + 1]),
                             reads=[("modc", L, sc_i), ("modc", L, sh_i)], writes=[("h32", rb, k), ("b", bk)])
                if extra is not None and u % 4 == 3:
                    next(extra, None)
            if router:
                for jj in range(4):
                    j = 4 * r + jj
                    lb = 6 + (j % 2)

                    def rmm(e, rb=rb, jj=jj, lb=lb):
                        for k in range(KC):
                            ins = e.matmul(banks[lb][:, 0:NE], lhsT=h32[:, rb, k, jj * 128:(jj + 1) * 128], rhs=rt[:, k, :], start=(k == 0), stop=(k == KC - 1))
                        return ins
                    P.op("pe", rmm, reads=[("h32", rb, k) for k in range(KC)] + ["rt"], writes=[("b", lb)])
                    P.op("dve", lambda e, j=j, lb=lb: e.tensor_copy(out=lg[:, j, :], in_=banks[lb][:, 0:NE]), writes=[("lg", j), ("b", lb)])
                    P.op("dve", lambda e, j=j: e.max(out=m8[:, j, :], in_=lg[:, j, :]), reads=[("lg", j)], writes=[("m8", j)])

        stage0(0)
        stage0(1)
        stage1(0)
        for r in range(NR):
            if r + 2 < NR:
                stage0(r + 2)
            if r + 1 < NR:
                stage1(r + 1)
            stage2(r)
        if extra is not None:
            for _ in extra:
                pass
        if router:
            allj = list(range(NT))
            P.op("dve", lambda e: e.tensor_tensor(out=ex[:], in0=lg[:], in1=m8[:, :, 0:1].to_broadcast([128, NT, NE]), op=ALU.subtract),
                 reads=[("lg", j) for j in allj] + [("m8", j) for j in allj], writes=["ex0"])
            P.op("act", lambda e: e.activation(out=ex[:], in_=ex[:], func=AF.Exp), reads=["ex0"], writes=["ex1"])
            P.op("dve", lambda e: e.tensor_tensor(out=lg[:], in0=lg[:], in1=m8[:, :, 1:2].to_broadcast([128, NT, NE]), op=ALU.is_ge),
                 reads=["ex0"], writes=["mask"])
            P.op("dve", lambda e: e.tensor_tensor(out=ex[:], in0=ex[:], in1=lg[:], op=ALU.mult), reads=["ex1", "mask"], writes=["ex2"])
            den = stat[:, 32:48]
            P.op("dve", lambda e: e.tensor_reduce(out=den, in_=ex[:], axis=mybir.AxisListType.X, op=ALU.add), reads=["ex2"], writes=["den0"])
            P.op("dve", lambda e: e.reciprocal(out=den, in_=den), reads=["den0"], writes=["den"])
            P.op("dve", lambda e: e.tensor_tensor(out=gates[:], in0=ex[:], in1=den.unsqueeze(2).to_broadcast([128, NT, NE]), op=ALU.mult),
                 reads=["ex2", "den"], writes=["gates"])
        barrier()

    def phase_ffn(experts, final=False):
        L = CL[0]
        gg_bc = gg_all[:, L]
        y_acc = R.alloc([128, NT, D])
        P.op("dve", lambda e: e.memset(stat[:, 0:64], 0.0), writes=["ss"])
        act = R.alloc([128, 2, T], BF16)
        w1b = [R.alloc([128, KC, FG], BF16) for _ in range(2)]
        w3b = [R.alloc([128, KC, FG], BF16) for _ in range(2)]
        w2b = [R.alloc([128, 2, D], BF16) for _ in range(2)]
        sg = [R.alloc([128, 512], BF16) for _ in range(2)]
        groups = [(w1, w3, w2, g, gi) for (w1, w3, w2, F, gi) in experts for g in range(F // FG)]
        NG = len(groups)
        cntA = [0]
        cntB = [0]

        def load(i):
            w1, w3, w2, g, gi = groups[i]
            s = i % 2
            P.op("pool", lambda e: e.dma_start(out=w1b[s][:], in_=w1[:, g * FG:(g + 1) * FG].rearrange("(k p) f -> p k f", p=128)),
                 writes=[("w1b", s)], dma=True)
            P.op("pool", lambda e: e.dma_start(out=w3b[s][:], in_=w3[:, g * FG:(g + 1) * FG].rearrange("(k p) f -> p k f", p=128)),
                 writes=[("w3b", s)], dma=True)
            P.op("pool", lambda e: e.dma_start(out=w2b[s][:], in_=w2[g * FG:(g + 1) * FG, :].rearrange("(c p) d -> p c d", p=128)),
                 writes=[("w2b", s)], dma=True)

        def A_unit(i, r, c):
            s = i % 2
            n = cntA[0]
            cntA[0] += 1
            pb = 2 * (n % 2)
            sgi = n % 2

            def mmA(e):
                for k in range(KC):
                    e.matmul(banks[pb][:], lhsT=w1b[s][:, k, c * 128:(c + 1) * 128], rhs=h_cm[:, k, r * 512:(r + 1) * 512],
                             start=(k == 0), stop=(k == KC - 1))
                for k in range(KC):
                    ins = e.matmul(banks[pb + 1][:], lhsT=w3b[s][:, k, c * 128:(c + 1) * 128], rhs=h_cm[:, k, r * 512:(r + 1) * 512],
                                   start=(k == 0), stop=(k == KC - 1))
                return ins
            P.op("pe", mmA, reads=[("w1b", s), ("w3b", s)] + hkeys(r), writes=[("b", pb), ("b", pb + 1)])
            P.op("act", lambda e: e.activation(out=sg[sgi][:], in_=banks[pb][:], func=AF.Silu), writes=[("sg", sgi), ("b", pb)])
            P.op("dve", lambda e: e.tensor_tensor(out=act[:, c, r * 512:(r + 1) * 512], in0=sg[sgi][:], in1=banks[pb + 1][:], op=ALU.mult),
                 reads=[("sg", sgi)], writes=[("act", c, r), ("b", pb + 1)])

        def B_unit(i, t, dh):
            w1, w3, w2, g, gi = groups[i]
            s = i % 2
            n = cntB[0]
            cntB[0] += 1
            yb = 4 + (n % 4)

            def mmB(e):
                for c in range(2):
                    ins = e.matmul(banks[yb][:], lhsT=act[:, c, t * 128:(t + 1) * 128], rhs=w2b[s][:, c, dh * 512:(dh + 1) * 512],
                                   start=(c == 0), stop=(c == 1))
                return ins
            P.op("pe", mmB, reads=[("act", 0, t // 4), ("act", 1, t // 4), ("w2b", s)], writes=[("b", yb)])
            ya = y_acc[:, t, dh * 512:(dh + 1) * 512]
            first = (i == 0)
            if gi is None:
                if first:
                    P.op("dve", lambda e: e.tensor_copy(out=ya, in_=banks[yb][:]), writes=[("y", t, dh), ("b", yb)])
                else:
                    P.op("dve", lambda e: e.tensor_tensor(out=ya, in0=banks[yb][:], in1=ya, op=ALU.add),
                         reads=[("y", t, dh)], writes=[("y", t, dh), ("b", yb)])
            else:
                gsc = gates[:, t, gi:gi + 1]
                if first:
                    P.op("dve", lambda e: e.tensor_scalar(out=ya, in0=banks[yb][:], scalar1=gsc, scalar2=None, op0=ALU.mult),
                         reads=["gates"], writes=[("y", t, dh), ("b", yb)])
                else:
                    P.op("dve", lambda e: e.scalar_tensor_tensor(out=ya, in0=banks[yb][:], scalar=gsc, in1=ya, op0=ALU.mult, op1=ALU.add),
                         reads=[("y", t, dh), "gates"], writes=[("y", t, dh), ("b", yb)])

        load(0)
        for r in range(NR):
            for c in range(2):
                A_unit(0, r, c)
        for i in range(NG):
            if i + 1 < NG:
                load(i + 1)
            for t in range(4):
                for dh in range(2):
                    B_unit(i, t, dh)
            for r in range(NR):
                for c in range(2):
                    if i + 1 < NG:
                        A_unit(i + 1, r, c)
                    if r + 1 < NR:
                        for t in (4 * (r + 1) + 2 * c, 4 * (r + 1) + 2 * c + 1):
                            for dh in range(2):
                                B_unit(i, t, dh)
        junk = sg[0]
        ss = stat[:, 0:16]
        rstd = stat[:, 16:32]
        def sq(g):
            for t in range(4 * g, 4 * g + 4):
                for dh in range(2):
                    P.op("act", lambda e, t=t, dh=dh: e.activation(out=junk[:], in_=y_acc[:, t, dh * 512:(dh + 1) * 512], func=AF.Square,
                                                                    accum_out=stat[:, 32 + 2 * t + dh:33 + 2 * t + dh]),
                         reads=[("y", t, dh), "ss"], writes=[("ssy", t, dh), ("sg", 0)])

        def fin(g):
            sl = slice(4 * g, 4 * g + 4)
            P.op("dve", lambda e: e.tensor_reduce(out=ss[:, sl], in_=stat[:, 32 + 8 * g:40 + 8 * g].rearrange("p (a b) -> p a b", b=2),
                                                  axis=mybir.AxisListType.X, op=ALU.add),
                 reads=[("ssy", t, dh) for t in range(4 * g, 4 * g + 4) for dh in range(2)] + ["ss"], writes=[("ss2", g)])
            P.op("act", lambda e: e.activation(out=rstd[:, sl], in_=ss[:, sl], func=AF.Sqrt, bias=NORM_EPS, scale=1.0 / D),
                 reads=[("ss2", g)], writes=[("rstd0", g)])
            P.op("dve", lambda e: e.reciprocal(out=rstd[:, sl], in_=rstd[:, sl]), reads=[("rstd0", g)], writes=[("rstdg", g)])

        def upd(g):
            for t in range(4 * g, 4 * g + 4):
                P.op("dve", lambda e, t=t: e.scalar_tensor_tensor(out=y_acc[:, t, :], in0=y_acc[:, t, :], scalar=rstd[:, t:t + 1], in1=gg_bc[:, 1, :],
                                                                   op0=ALU.mult, op1=ALU.mult),
                     reads=[("y", t, 0), ("y", t, 1), ("rstdg", g), ("gg", L, 1, 0), ("gg", L, 1, 1)], writes=[("y", t, 0), ("y", t, 1)])
                P.op("dve" if t % 4 else "pool", lambda e, t=t: e.tensor_tensor(out=x_tm[:, t, :], in0=x_tm[:, t, :], in1=y_acc[:, t, :], op=ALU.add),
                     reads=[("y", t, 0), ("y", t, 1), ("x", t)], writes=[("x", t)])
                if final:
                    P.op("sp", lambda e, t=t: e.dma_start(out=out_d[t * 128:(t + 1) * 128, :], in_=x_tm[:, t, :]), reads=[("x", t)], dma=True)
                    out_done[0] = True

        sq(0)
        sq(1)
        for g in range(4):
            fin(g)
            if g + 2 < 4:
                sq(g + 2)
            upd(g)
        barrier()


    CL = [0]
    out_done = [False]

    def hkeys(r):
        return [("h", k, j) for k in range(KC) for j in range(4 * r, 4 * r + 4)]

    def barrier_keep(m):
        barrier()
        R.release(m)

    def evac_copy(i, out, in_, reads, writes):
        if i % 2 == 0:
            P.op("act", lambda e: e.activation(out=out, in_=in_, func=AF.Copy), reads=reads, writes=writes)
        else:
            P.op("dve", lambda e: e.tensor_copy(out=out, in_=in_), reads=reads, writes=writes)

    def phase_out(w_out_ap, cat_k, extra=None):
        L = CL[0]
        gg_bc = gg_all[:, L]
        wo = R.alloc([128, KC, D], BF16)
        tmp = R.alloc([128, 2, D])
        junk = R.alloc([128, 512], BF16)
        for kh in range(2):
            P.op("pool", lambda e, kh=kh: e.dma_start(out=wo[:, kh * 4:(kh + 1) * 4, :],
                                                       in_=w_out_ap[kh * 512:(kh + 1) * 512, :].rearrange("(k p) d -> p k d", p=128)),
                 writes=[("wo", kh)], dma=True)
        P.op("dve", lambda e: e.memset(stat[:, 0:64], 0.0), writes=["sso"])
        for j in range(NT):
            b0 = 2 * (j % 2)

            def mm(e, j=j, b0=b0):
                for dh in range(2):
                    for k in range(KC):
                        ins = e.matmul(banks[b0 + dh][:], lhsT=cat_k(k)[:, j * 128:(j + 1) * 128], rhs=wo[:, k, dh * 512:(dh + 1) * 512],
                                       start=(k == 0), stop=(k == KC - 1))
                return ins
            P.op("pe", mm, reads=[("wo", 0), ("wo", 1)] + [("cat", k, j // 4) for k in range(KC)], writes=[("b", b0), ("b", b0 + 1)])
            for dh in range(2):
                P.op("act", lambda e, j=j, dh=dh, b0=b0: e.activation(out=junk[:], in_=banks[b0 + dh][:], func=AF.Square,
                                                                       accum_out=stat[:, 32 + 2 * j + dh:33 + 2 * j + dh]),
                     reads=["sso"], writes=[("b", b0 + dh), ("ssq", j, dh), "junk_o"])
            P.op("dve", lambda e, j=j: e.tensor_tensor(out=stat[:, j:j + 1], in0=stat[:, 32 + 2 * j:33 + 2 * j], in1=stat[:, 33 + 2 * j:34 + 2 * j], op=ALU.add),
                 reads=[("ssq", j, 0), ("ssq", j, 1)], writes=[("sso1", j)])
            P.op("act", lambda e, j=j: e.activation(out=stat[:, 16 + j:17 + j], in_=stat[:, j:j + 1], func=AF.Sqrt, bias=NORM_EPS, scale=1.0 / D),
                 reads=[("sso1", j)], writes=[("sso2", j)])
            P.op("dve", lambda e, j=j: e.reciprocal(out=stat[:, 16 + j:17 + j], in_=stat[:, 16 + j:17 + j]), reads=[("sso2", j)], writes=[("rstdo", j)])
            for dh in range(2):
                sl = slice(dh * 512, (dh + 1) * 512)
                P.op("dve", lambda e, j=j, dh=dh, sl=sl, b0=b0: e.scalar_tensor_tensor(
                    out=tmp[:, j % 2, sl], in0=banks[b0 + dh][:], scalar=stat[:, 16 + j:17 + j], in1=gg_bc[:, 0, sl], op0=ALU.mult, op1=ALU.mult),
                    reads=[("rstdo", j), ("gg", L, 0, dh)], writes=[("b", b0 + dh), ("tmpo", j % 2, dh)])
            P.op("dve" if j % 4 else "pool", lambda e, j=j: e.tensor_tensor(out=x_tm[:, j, :], in0=x_tm[:, j, :], in1=tmp[:, j % 2, :], op=ALU.add),
                 reads=[("tmpo", j % 2, 0), ("tmpo", j % 2, 1)], writes=[("x", j)])
            if extra is not None:
                next(extra, None)
        if extra is not None:
            for _ in extra:
                pass
        barrier()

    def phase_mix1():
        cat = R.alloc([128, KC, T], BF16)
        m0 = R.mark()
        AB = R.alloc([128, NT, 4, 256], BF16)
        fT = R.alloc([128, 2, 4, 512], BF16)
        wf = R.alloc([128, KC, 512], BF16)
        cs = R.alloc([128, 256], BF16)
        dblk = [R.alloc([128, 2, 512], BF16) for _ in range(6)]
        P.op("pool", lambda e: e.dma_start(out=wf[:], in_=cd_w_in[:, 1024:1536].rearrange("(k p) f -> p k f", p=128)), writes=["wf"], dma=True)
        P.op("sp", lambda e: e.dma_start(out=cs[:], in_=cs128_d), writes=["cs"], dma=True)
        for r in range(NR):
            rb = r % 2
            for g in range(4):
                bk = g % 2

                def mm(e, r=r, g=g, bk=bk):
                    for k in range(KC):
                        ins = e.matmul(banks[bk][:], lhsT=wf[:, k, g * 128:(g + 1) * 128], rhs=h_cm[:, k, r * 512:(r + 1) * 512],
                                       start=(k == 0), stop=(k == KC - 1))
                    return ins
                P.op("pe", mm, reads=["wf"] + hkeys(r), writes=[("b", bk)])
                evac_copy(g, fT[:, rb, g, :], banks[bk][:], [], [("fT", rb, g), ("b", bk)])
            for jj in range(4):
                j = 4 * r + jj
                b2 = 2 + 2 * (jj % 2)

                def mm2(e, rb=rb, jj=jj, b2=b2):
                    for g in range(4):
                        ins = e.matmul(banks[b2 + g // 2][:, (g % 2) * 256:(g % 2 + 1) * 256], lhsT=fT[:, rb, g, jj * 128:(jj + 1) * 128], rhs=cs[:],
                                       start=True, stop=True)
                    return ins
                P.op("pe", mm2, reads=[("fT", rb, g) for g in range(4)] + ["cs"], writes=[("b", b2), ("b", b2 + 1)])
                for hh in range(2):
                    evac_copy(hh, AB[:, j, 2 * hh:2 * hh + 2, :], banks[b2 + hh][:].rearrange("p (a b) -> p a b", b=256), [],
                              [("AB", j, hh), ("b", b2 + hh)])
        nblk = 0
        for r in range(NR):
            bs = 4 * (r % 2)
            for j in range(NT):
                sl = nblk % 6
                nblk += 1
                P.op("sp", lambda e, r=r, j=j, sl=sl: e.dma_start(out=dblk[sl][:], in_=dft_d[r, j]), writes=[("dblk", sl)], dma=True)

                def mm3(e, j=j, sl=sl, bs=bs):
                    for g in range(4):
                        for c2 in range(2):
                            ins = e.matmul(banks[bs + g][:], lhsT=AB[:, j, g, c2 * 128:(c2 + 1) * 128], rhs=dblk[sl][:, c2, :],
                                           start=(j == 0 and c2 == 0), stop=(j == NT - 1 and c2 == 1))
                    return ins
                P.op("pe", mm3, reads=[("dblk", sl), ("AB", j, 0), ("AB", j, 1)], writes=[("b", bs + g) for g in range(4)])
            for g in range(4):
                evac_copy(g, cat[:, 4 + g, r * 512:(r + 1) * 512], banks[bs + g][:], [], [("cat", 4 + g, r), ("b", bs + g)])
        barrier_keep(m0)
        v_tm = R.alloc([128, NT, 512], BF16)
        wv = R.alloc([128, KC, 512], BF16)
        wu = R.alloc([128, KC, 512], BF16)
        wsr = R.alloc([128, 8, 128])
        wsT = R.alloc([128, 8, 128], BF16)
        bsb = R.alloc([128, 4, 128])
        lnr = R.alloc([128, 2, 512])
        vg = R.alloc([128, 2, 512])
        ug = R.alloc([128, 2, 512])
        tmp2 = R.alloc([128, 2, 512])
        junk = R.alloc([128, 512], BF16)
        st2 = R.alloc([128, NT, 8])
        P.op("pool", lambda e: e.dma_start(out=wv[:], in_=cd_w_in[:, 512:1024].rearrange("(k p) f -> p k f", p=128)), writes=["wv"], dma=True)
        P.op("pool", lambda e: e.dma_start(out=wu[:], in_=cd_w_in[:, 0:512].rearrange("(k p) f -> p k f", p=128)), writes=["wu"], dma=True)
        P.op("sp", lambda e: e.dma_start(out=wsr[:], in_=sg_w_s.rearrange("h p q -> p h q")), writes=["wsr"], dma=True)
        for h in range(8):
            P.op("sp", lambda e, h=h: e.dma_start(out=bsb[(h % 2) * 64:(h % 2 + 1) * 64, h // 2, :], in_=sg_b_s[h:h + 1, :].to_broadcast([64, 128])),
                 writes=[("bsb", h)], dma=True)
        P.op("sp", lambda e: e.dma_start(out=lnr[:, 0, :], in_=sg_ln_g.to_broadcast([128, 512])), writes=[("lnr", 0)], dma=True)
        P.op("sp", lambda e: e.dma_start(out=lnr[:, 1, :], in_=sg_ln_b.to_broadcast([128, 512])), writes=[("lnr", 1)], dma=True)
        for hf in range(2):
            def trw(e, hf=hf):
                for hq in range(4):
                    ins = e.transpose(out=banks[hf][:, hq * 128:(hq + 1) * 128], in_=wsr[:, hf * 4 + hq, :], identity=ident[:])
                return ins
            P.op("pe", trw, reads=["wsr", "ident"], writes=[("b", hf)])
            evac_copy(hf, wsT[:, hf * 4:hf * 4 + 4, :], banks[hf][:].rearrange("p (a b) -> p a b", b=128), [], [("wsT", hf), ("b", hf)])
        P.op("dve", lambda e: e.memset(st2[:], 0.0), writes=["st2"])
        st3 = st2.rearrange("p a b -> p (a b)").rearrange("p (a b) -> p a b", b=NT)
        for j in range(NT):
            vb = 4 + (j % 2)
            jb = j % 2

            def mmv(e, j=j, vb=vb):
                for k in range(KC):
                    ins = e.matmul(banks[vb][:], lhsT=h_cm[:, k, j * 128:(j + 1) * 128], rhs=wv[:, k, :], start=(k == 0), stop=(k == KC - 1))
                return ins
            P.op("pe", mmv, reads=["wv"] + [("h", k, j) for k in range(KC)], writes=[("b", vb)])
            P.op("act", lambda e, j=j, vb=vb, jb=jb: e.activation(out=vg[:, jb, :], in_=banks[vb][:], func=AF.Gelu_apprx_tanh, accum_out=st3[:, 0, j:j + 1]),
                 reads=["st2"], writes=[("vg", jb), ("b", vb), ("st", j, 0)])
            P.op("act", lambda e, j=j, jb=jb: e.activation(out=junk[:], in_=vg[:, jb, :], func=AF.Square, accum_out=st3[:, 1, j:j + 1]),
                 reads=[("vg", jb), "st2"], writes=[("st", j, 1), "junk1"])
            P.op("dve", lambda e, j=j, jb=jb: e.tensor_copy(out=v_tm[:, j, :], in_=vg[:, jb, :]), reads=[("vg", jb)], writes=[("v", j)])
        allst = [("st", j, i) for j in range(NT) for i in range(2)]
        P.op("dve", lambda e: e.tensor_scalar(out=st3[:, 2:4, :], in0=st3[:, 0:2, :], scalar1=1.0 / 512, scalar2=None, op0=ALU.mult), reads=allst, writes=["stm"])
        P.op("dve", lambda e: e.tensor_tensor(out=st3[:, 4, :], in0=st3[:, 2, :], in1=st3[:, 2, :], op=ALU.mult), reads=["stm"], writes=["stq"])
        P.op("dve", lambda e: e.tensor_tensor(out=st3[:, 4, :], in0=st3[:, 4, :], in1=st3[:, 3, :], op=ALU.subtract), reads=["stm", "stq"], writes=["stv"])
        P.op("act", lambda e: e.activation(out=st3[:, 5, :], in_=st3[:, 4, :], func=AF.Sqrt, bias=LN_EPS, scale=-1.0), reads=["stv"], writes=["std"])
        P.op("dve", lambda e: e.reciprocal(out=st3[:, 6, :], in_=st3[:, 5, :]), reads=["std"], writes=["str"])
        P.op("dve", lambda e: e.scalar_tensor_tensor(out=st3[:, 7, :], in0=st3[:, 2, :], scalar=-1.0, in1=st3[:, 6, :], op0=ALU.mult, op1=ALU.mult),
             reads=["stm", "str"], writes=["stb"])
        for r in range(NR):
            for j in range(4 * r, 4 * r + 4):
                jb = j % 2
                P.op("act", lambda e, j=j, jb=jb: e.activation(out=vg[:, jb, :], in_=v_tm[:, j, :], func=AF.Identity, bias=st3[:, 7, j:j + 1], scale=st3[:, 6, j:j + 1]),
                     reads=["stb", "str", ("v", j)], writes=[("vg", jb)])
                P.op("dve", lambda e, jb=jb: e.tensor_tensor(out=vg[:, jb, :], in0=vg[:, jb, :], in1=lnr[:, 0, :], op=ALU.mult),
                     reads=[("lnr", 0)], writes=[("vg", jb)])
                P.op("dve", lambda e, j=j, jb=jb: e.tensor_tensor(out=v_tm[:, j, :], in0=vg[:, jb, :], in1=lnr[:, 1, :], op=ALU.add),
                     reads=[("lnr", 1), ("vg", jb)], writes=[("v", j)])
            for cc in range(4):
                i2 = (r * 4 + cc) % 2

                def mmu(e, cc=cc, r=r, i2=i2):
                    for k in range(KC):
                        ins = e.matmul(banks[i2][:], lhsT=wu[:, k, cc * 128:(cc + 1) * 128], rhs=h_cm[:, k, r * 512:(r + 1) * 512],
                                       start=(k == 0), stop=(k == KC - 1))
                    return ins
                P.op("pe", mmu, reads=["wu"] + hkeys(r), writes=[("b", i2)])
                P.op("act", lambda e, i2=i2: e.activation(out=ug[:, i2, :], in_=banks[i2][:], func=AF.Gelu_apprx_tanh), writes=[("ug", i2), ("b", i2)])

                def mmm(e, cc=cc, r=r, i2=i2):
                    for nl in range(4):
                        for hh in range(2):
                            h = 2 * cc + hh
                            ins = e.matmul(banks[2 + i2][hh * 64:(hh + 1) * 64, nl * 128:(nl + 1) * 128], lhsT=v_tm[:, 4 * r + nl, h * 64:(h + 1) * 64],
                                           rhs=wsT[:, h, :], start=True, stop=True)
                    return ins
                P.op("pe", mmm, reads=[("v", 4 * r + nl) for nl in range(4)] + [("wsT", 0), ("wsT", 1)], writes=[("b", 2 + i2)])
                P.op("dve", lambda e, cc=cc, i2=i2: e.tensor_tensor(
                    out=tmp2[:, i2, :].rearrange("p (a b) -> p a b", b=128), in0=banks[2 + i2][:].rearrange("p (a b) -> p a b", b=128),
                    in1=bsb[:, cc, :].unsqueeze(1).to_broadcast([128, 4, 128]), op=ALU.add),
                    reads=[("bsb", 2 * cc), ("bsb", 2 * cc + 1)], writes=[("tmp2", i2), ("b", 2 + i2)])
                P.op("dve", lambda e, cc=cc, r=r, i2=i2: e.tensor_tensor(out=cat[:, cc, r * 512:(r + 1) * 512], in0=tmp2[:, i2, :], in1=ug[:, i2, :], op=ALU.mult),
                     reads=[("tmp2", i2), ("ug", i2)], writes=[("cat", cc, r)])
        barrier_keep(m0)
        phase_out(cd_w_out, lambda k: cat[:, k, :])

    def phase_mix0():
        catB = R.alloc([128, 4, T], BF16)
        mB = R.mark()
        wB = [R.alloc([128, KC, 256], BF16) for _ in range(2)]
        glu = R.alloc([128, 4, T + 30], BF16)
        dg = R.alloc([128, 4, 31, 128], BF16)
        upre = R.alloc([128, 4, 512])
        ub = R.alloc([128, 4, 512], BF16)
        usq = R.alloc([128, 4, 512], BF16)
        sgt = R.alloc([128, 2, 512])
        t1 = R.alloc([128, 2, 512])
        sbc = R.alloc([128, 2, 512])
        ident_bf = R.alloc([128, 128], BF16)
        ones_bf = R.alloc([128, 128], BF16)
        cw0 = colmap["cv_dw_w"]
        cb0 = colmap["cv_dw_b"]
        lg0 = colmap["cv_ln_g"]
        lb0 = colmap["cv_ln_b"]
        P.op("dve", lambda e: e.tensor_copy(out=ident_bf[:], in_=ident[:]), reads=["ident"], writes=["ident_bf"])
        P.op("dve", lambda e: e.memset(ones_bf[:], 1.0 / 512), writes=["ones_bf"])
        P.op("dve", lambda e: e.memset(glu[:], 0.0), writes=["glu0"])
        for cc in range(4):
            for j in range(31):
                P.op("act" if j % 3 else "dve", lambda e, cc=cc, j=j: (e.activation(out=dg[:, cc, j, :], in_=ident_bf[:], func=AF.Copy, scale=cols[:, cw0 + j * 4 + cc:cw0 + j * 4 + cc + 1])
                                                                       if j % 3 else
                                                                       e.tensor_scalar(out=dg[:, cc, j, :], in0=ident_bf[:], scalar1=cols[:, cw0 + j * 4 + cc:cw0 + j * 4 + cc + 1], scalar2=None, op0=ALU.mult)),
                     reads=["ident_bf", "cols"], writes=[("dg", cc, j)])
        for cc in range(4):
            slot = cc % 2
            P.op("pool", lambda e, cc=cc, slot=slot: e.dma_start(out=wB[slot][:, :, 0:128],
                                                                  in_=ab_w_in[:, 1024 + cc * 128:1024 + (cc + 1) * 128].rearrange("(k p) f -> p k f", p=128)),
                 writes=[("wB", slot, 0)], dma=True)
            P.op("pool", lambda e, cc=cc, slot=slot: e.dma_start(out=wB[slot][:, :, 128:256],
                                                                  in_=ab_w_in[:, 1536 + cc * 128:1536 + (cc + 1) * 128].rearrange("(k p) f -> p k f", p=128)),
                 writes=[("wB", slot, 1)], dma=True)
            for r in range(NR):
                i2 = (cc * NR + r) % 2
                pb = 2 * i2

                def mmg(e, slot=slot, r=r, pb=pb):
                    for k in range(KC):
                        e.matmul(banks[pb][:], lhsT=wB[slot][:, k, 0:128], rhs=h_cm[:, k, r * 512:(r + 1) * 512], start=(k == 0), stop=(k == KC - 1))
                    for k in range(KC):
                        ins = e.matmul(banks[pb + 1][:], lhsT=wB[slot][:, k, 128:256], rhs=h_cm[:, k, r * 512:(r + 1) * 512], start=(k == 0), stop=(k == KC - 1))
                    return ins
                P.op("pe", mmg, reads=[("wB", slot, 0), ("wB", slot, 1)] + hkeys(r), writes=[("b", pb), ("b", pb + 1)])
                P.op("act", lambda e, pb=pb, i2=i2: e.activation(out=sgt[:, i2, :], in_=banks[pb + 1][:], func=AF.Sigmoid), writes=[("sgt", i2), ("b", pb + 1)])
                P.op("dve", lambda e, cc=cc, r=r, pb=pb, i2=i2: e.tensor_tensor(out=glu[:, cc, 15 + r * 512:15 + (r + 1) * 512], in0=banks[pb][:], in1=sgt[:, i2, :], op=ALU.mult),
                     reads=[("sgt", i2), "glu0"], writes=[("glu", cc, r), ("b", pb)])
        for r in range(NR):
            for cc in range(4):
                cb = 4 + (cc % 2)

                def mmc(e, cc=cc, r=r, cb=cb):
                    for j in range(31):
                        ins = e.matmul(banks[cb][:], lhsT=dg[:, cc, j, :], rhs=glu[:, cc, r * 512 + j:r * 512 + j + 512], start=(j == 0), stop=(j == 30))
                    return ins
                P.op("pe", mmc, reads=[("dg", cc, jj) for jj in range(31)] + ["glu0"] + [("glu", cc, rr) for rr in range(max(0, r - 1), min(NR, r + 2))], writes=[("b", cb)])
                P.op("act", lambda e, cc=cc, cb=cb: e.activation(out=upre[:, cc, :], in_=banks[cb][:], func=AF.Identity, bias=cols[:, cb0 + cc:cb0 + cc + 1]),
                     reads=["cols"], writes=[("upre", cc), ("b", cb)])
                P.op("dve", lambda e, cc=cc: e.tensor_copy(out=ub[:, cc, :], in_=upre[:, cc, :]), reads=[("upre", cc)], writes=[("ub", cc)])
                P.op("act", lambda e, cc=cc: e.activation(out=usq[:, cc, :], in_=upre[:, cc, :], func=AF.Square), reads=[("upre", cc)], writes=[("usq", cc)])

            def mms(e):
                for cc in range(4):
                    e.matmul(banks[6][:], lhsT=ones_bf[:], rhs=ub[:, cc, :], start=(cc == 0), stop=(cc == 3))
                for cc in range(4):
                    ins = e.matmul(banks[7][:], lhsT=ones_bf[:], rhs=usq[:, cc, :], start=(cc == 0), stop=(cc == 3))
                return ins
            P.op("pe", mms, reads=["ones_bf"] + [("ub", cc) for cc in range(4)] + [("usq", cc) for cc in range(4)], writes=[("b", 6), ("b", 7)])
            P.op("act", lambda e: e.activation(out=sbc[:, 0, :], in_=banks[6][:], func=AF.Square), writes=[("sbc", 0), ("b", 6)])
            P.op("dve", lambda e: e.tensor_tensor(out=sbc[:, 0, :], in0=sbc[:, 0, :], in1=banks[7][:], op=ALU.subtract), writes=[("sbc", 0), ("b", 7)])
            P.op("act", lambda e: e.activation(out=sbc[:, 1, :], in_=sbc[:, 0, :], func=AF.Sqrt, bias=LN_EPS, scale=-1.0), reads=[("sbc", 0)], writes=[("sbc", 1)])
            P.op("dve", lambda e: e.reciprocal(out=sbc[:, 1, :], in_=sbc[:, 1, :]), writes=[("sbc", 1)])
            for cc in range(4):
                tb = cc % 2
                P.op("dve", lambda e, cc=cc, tb=tb: e.tensor_tensor(out=t1[:, tb, :], in0=upre[:, cc, :], in1=banks[6][:], op=ALU.subtract),
                     reads=[("upre", cc)], writes=[("t1", tb), ("b", 6)])
                P.op("dve", lambda e, tb=tb: e.tensor_tensor(out=t1[:, tb, :], in0=t1[:, tb, :], in1=sbc[:, 1, :], op=ALU.mult),
                     reads=[("sbc", 1)], writes=[("t1", tb)])
                P.op("act", lambda e, cc=cc, r=r, tb=tb: e.activation(out=catB[:, cc, r * 512:(r + 1) * 512], in_=t1[:, tb, :], func=AF.Silu,
                                                                       bias=cols[:, lb0 + cc:lb0 + cc + 1], scale=cols[:, lg0 + cc:lg0 + cc + 1]),
                     reads=[("t1", tb), "cols"], writes=[("cat", 4 + cc, r)])
        barrier_keep(mB)
        catA = R.alloc([128, 4, T], BF16)
        mA = R.mark()
        seg_t = [R.alloc([128, 2, 512]) for _ in range(5)]
        Rt, At, It, Hb, Gt = seg_t
        ct = reg_t[:, mA:mA + 2048].rearrange("p (a b) -> p a b", b=D)
        wA = [R.alloc([128, KC, 256], BF16) for _ in range(2)]
        xpp = R.alloc([128, 2310], BF16)
        XAb = R.alloc([128, 2, CTX + T], BF16)
        Hf = R.alloc([128, T])
        Hc = R.alloc([128, 2, CTX])
        gw = R.alloc([128, 16, 128], BF16)
        dg4 = R.alloc([128, 4, 4, 128], BF16)
        hc_cm = R.alloc([128, KC, CTX], BF16)
        clc = R.alloc([128, 32])
        ncol = R.alloc([128, 16])
        ident_bf = R.alloc([128, 128], BF16)
        junkc = R.alloc([128, D], BF16)
        c4w = colmap["rg_conv_w"]
        c4b = colmap["rg_conv_b"]
        cbr = colmap["rg_b_r"]
        cbi = colmap["rg_b_i"]
        clam = colmap["rg_lambda"]
        ckeys = [("seg", 0, 0), ("seg", 0, 1), ("seg", 1, 0), ("seg", 1, 1)]
        P.op("dve", lambda e: e.memset(stat[:, 0:64], 0.0), writes=["ssc"])
        for jc in range(2):
            P.op("sp", lambda e, jc=jc: e.dma_start(out=ct[:, jc, :], in_=ctx_d[jc * 128:(jc + 1) * 128, :]), writes=ckeys[2 * jc:2 * jc + 2], dma=True)
            P.op("act", lambda e, jc=jc: e.activation(out=junkc[:], in_=ct[:, jc, :], func=AF.Square, accum_out=stat[:, jc:jc + 1]),
                 reads=ckeys[2 * jc:2 * jc + 2] + ["ssc"], writes=[("ssc1", jc), "junkc"])
        P.op("act", lambda e: e.activation(out=stat[:, 16:18], in_=stat[:, 0:2], func=AF.Sqrt, bias=NORM_EPS, scale=1.0 / D),
             reads=[("ssc1", 0), ("ssc1", 1)], writes=["ssc2"])
        P.op("dve", lambda e: e.reciprocal(out=stat[:, 16:18], in_=stat[:, 16:18]), reads=["ssc2"], writes=["rstdc"])
        for jc in range(2):
            P.op("act", lambda e, jc=jc: e.activation(out=ct[:, jc, :], in_=ct[:, jc, :], func=AF.Copy, scale=stat[:, 16 + jc:17 + jc]),
                 reads=["rstdc", ("ssc1", jc)], writes=ckeys[2 * jc:2 * jc + 2])
            b0 = 2 * jc

            def trc(e, jc=jc, b0=b0):
                for k in range(KC):
                    ins = e.transpose(out=banks[b0 + k // 4][:, (k % 4) * 128:(k % 4 + 1) * 128], in_=ct[:, jc, k * 128:(k + 1) * 128], identity=ident[:])
                return ins
            P.op("pe", trc, reads=ckeys[2 * jc:2 * jc + 2] + ["ident"], writes=[("b", b0), ("b", b0 + 1)])
            for k in range(KC):
                src = banks[b0 + k // 4][:, (k % 4) * 128:(k % 4 + 1) * 128]
                if k < 4:
                    P.op("dve", lambda e, k=k, jc=jc, src=src: e.tensor_scalar(out=hc_cm[:, k, jc * 128:(jc + 1) * 128], in0=src, scalar1=modc_all[:, 0, 2, k:k + 1],
                                                                                scalar2=modc_all[:, 0, 3, k:k + 1], op0=ALU.mult, op1=ALU.add),
                         reads=[("modc", 0, 2), ("modc", 0, 3)], writes=[("hc", k, jc), ("b", b0 + k // 4)])
                else:
                    P.op("act", lambda e, k=k, jc=jc, src=src: e.activation(out=hc_cm[:, k, jc * 128:(jc + 1) * 128], in_=src, func=AF.Identity,
                                                                             bias=modc_all[:, 0, 3, k:k + 1], scale=modc_all[:, 0, 2, k:k + 1]),
                         reads=[("modc", 0, 2), ("modc", 0, 3)], writes=[("hc", k, jc), ("b", b0 + k // 4)])
        hckeys = [("hc", k, jc) for k in range(KC) for jc in range(2)]
        P.op("dve", lambda e: e.tensor_copy(out=ident_bf[:], in_=ident[:]), reads=["ident"], writes=["ident_bf"])
        P.op("dve", lambda e: e.memset(gw[:], 0.0), writes=["gw0"])
        P.op("dve", lambda e: e.memset(xpp[:], 0.0), writes=["xpp0"])
        for d in range(2):
            for ri, wsrc in enumerate((rg_w_r, rg_w_i)):
                for hh in range(2):
                    src = wsrc[d].rearrange("(c two) p q -> two p c q", two=2)[hh]
                    P.op("pool", lambda e, d=d, ri=ri, hh=hh, src=src: e.dma_start(
                        out=gw[hh * 64:(hh + 1) * 64, (d * 2 + ri) * 4:(d * 2 + ri) * 4 + 4, hh * 64:(hh + 1) * 64], in_=src),
                        reads=["gw0"], writes=[("gw", d, ri, cc, hh) for cc in range(4)], dma=True)
        for cc in range(4):
            for j in range(4):
                P.op("act", lambda e, cc=cc, j=j: e.activation(out=dg4[:, cc, j, :], in_=ident_bf[:], func=AF.Copy, scale=cols[:, c4w + j * 4 + cc:c4w + j * 4 + cc + 1]),
                     reads=["ident_bf", "cols"], writes=[("dg4", cc, j)])
        P.op("dve", lambda e: e.tensor_scalar(out=ncol[:, 0:8], in0=cols[:, cbr:cbr + 8], scalar1=0.5, scalar2=None, op0=ALU.mult), reads=["cols"], writes=["ncol"])
        P.op("dve", lambda e: e.tensor_scalar(out=ncol[:, 8:16], in0=cols[:, cbi:cbi + 8], scalar1=0.5, scalar2=None, op0=ALU.mult), reads=["cols"], writes=["ncol"])
        P.op("act", lambda e: e.activation(out=clc[:, 16:24], in_=cols[:, clam:clam + 8], func=AF.Exp, scale=-1.0), reads=["cols"], writes=["cl0"])
        P.op("act", lambda e: e.activation(out=clc[:, 24:32], in_=clc[:, 16:24], func=AF.Ln, bias=1.0), reads=["cl0"], writes=["cl1"])
        P.op("dve", lambda e: e.tensor_scalar(out=clc[:, 0:8], in0=clc[:, 24:32], scalar1=-4.0, scalar2=None, op0=ALU.mult), reads=["cl1"], writes=["cl"])
        P.op("dve", lambda e: e.tensor_scalar(out=clc[:, 8:16], in0=clc[:, 24:32], scalar1=-16.0, scalar2=None, op0=ALU.mult), reads=["cl1"], writes=["cl2"])
        segs = [("c", CTX, 0)] + [(r, 512, CTX + r * 512) for r in range(NR)]
        nev = [0]
        for cc in range(4):
            slot = cc % 2
            xb = cc % 2
            P.op("pool", lambda e, cc=cc, slot=slot: e.dma_start(out=wA[slot][:, :, 0:128],
                                                                  in_=ab_w_in[:, cc * 128:(cc + 1) * 128].rearrange("(k p) f -> p k f", p=128)),
                 writes=[("wA", slot, 0)], dma=True)
            P.op("pool", lambda e, cc=cc, slot=slot: e.dma_start(out=wA[slot][:, :, 128:256],
                                                                  in_=ab_w_in[:, 512 + cc * 128:512 + (cc + 1) * 128].rearrange("(k p) f -> p k f", p=128)),
                 writes=[("wA", slot, 1)], dma=True)
            for si, (sid, N, off) in enumerate(segs):
                bk = si % 2
                rk = hckeys if sid == "c" else hkeys(sid)
                xo = 1 if sid == "c" else 260 + sid * 512

                def mmx(e, slot=slot, sid=sid, N=N, bk=bk):
                    for k in range(KC):
                        rhs = hc_cm[:, k, :] if sid == "c" else h_cm[:, k, sid * 512:(sid + 1) * 512]
                        ins = e.matmul(banks[bk][:, 0:N], lhsT=wA[slot][:, k, 0:128], rhs=rhs, start=(k == 0), stop=(k == KC - 1))
                    return ins
                P.op("pe", mmx, reads=[("wA", slot, 0)] + rk, writes=[("b", bk)])
                nev[0] += 1
                evac_copy(nev[0], xpp[:, xo:xo + N], banks[bk][:, 0:N], ["xpp0"], [("xpp", sid), ("b", bk)])
            for si, (sid, N, off) in enumerate(segs):
                bk = 2 + si % 2
                base = 0 if sid == "c" else 259 + sid * 512
                nb = [("xpp", "c")] if sid == "c" else [("xpp", rr) for rr in range(max(0, sid - 1), min(NR, sid + 2))]

                def mm4(e, cc=cc, N=N, bk=bk, base=base):
                    for j in range(4):
                        ins = e.matmul(banks[bk][:, 0:N], lhsT=dg4[:, cc, j, :], rhs=xpp[:, base + j:base + j + N], start=(j == 0), stop=(j == 3))
                    return ins
                P.op("pe", mm4, reads=[("dg4", cc, jj) for jj in range(4)] + ["xpp0"] + nb, writes=[("b", bk)])
                P.op("act", lambda e, cc=cc, N=N, off=off, bk=bk, xb=xb: e.activation(out=XAb[:, xb, off:off + N], in_=banks[bk][:, 0:N], func=AF.Identity,
                                                                                       bias=cols[:, c4b + cc:c4b + cc + 1]),
                     reads=["cols"], writes=[("xa", xb, sid), ("b", bk)])
            for d in range(2):
                order = segs if d == 0 else [segs[0]] + segs[:0:-1]
                col = d * 4 + cc
                state = {"carry": None, "ckey": None}

                def seg_front(si, sid, N, off, d=d, cc=cc, col=col, xb=xb):
                    i2 = si % 2
                    bk = 4 + 2 * i2
                    xa = XAb[:, xb, off:off + N]

                    def mmg2(e):
                        e.matmul(banks[bk][:, 0:N], lhsT=gw[:, (d * 2 + 0) * 4 + cc, :], rhs=xa, start=True, stop=True)
                        return e.matmul(banks[bk + 1][:, 0:N], lhsT=gw[:, (d * 2 + 1) * 4 + cc, :], rhs=xa, start=True, stop=True)
                    P.op("pe", mmg2, reads=[("xa", xb, sid)] + [("gw", d, ri, cc, hh) for ri in range(2) for hh in range(2)] + ["gw0"],
                         writes=[("b", bk), ("b", bk + 1)])
                    rr_, ii_, aa_ = Rt[:, i2, 0:N], It[:, i2, 0:N], At[:, i2, 0:N]
                    kR, kI, kA = ("seg", 0, i2), ("seg", 2, i2), ("seg", 1, i2)
                    P.op("act", lambda e: e.activation(out=rr_, in_=banks[bk][:, 0:N], func=AF.Tanh, scale=0.5, bias=ncol[:, col:col + 1]),
                         reads=["ncol"], writes=[kR, ("b", bk)])
                    P.op("act", lambda e: e.activation(out=ii_, in_=banks[bk + 1][:, 0:N], func=AF.Tanh, scale=0.5, bias=ncol[:, 8 + col:9 + col]),
                         reads=["ncol"], writes=[kI, ("b", bk + 1)])
                    P.op("act", lambda e: e.activation(out=aa_, in_=rr_, func=AF.Exp, scale=clc[:, col:col + 1], bias=clc[:, col:col + 1]),
                         reads=[kR, "cl"], writes=[kA])
                    P.op("dve", lambda e: e.tensor_tensor(out=rr_, in0=aa_, in1=aa_, op=ALU.mult), reads=[kA], writes=[kR])
                    P.op("dve", lambda e: e.scalar_tensor_tensor(out=ii_, in0=ii_, scalar=1.0, in1=xa, op0=ALU.add, op1=ALU.mult),
                         reads=[("xa", xb, sid)], writes=[kI])

                def seg_back(si, sid, N, off, d=d, cc=cc, col=col, xb=xb, state=state):
                    i2 = si % 2
                    rr_, ii_, aa_ = Rt[:, i2, 0:N], It[:, i2, 0:N], At[:, i2, 0:N]
                    kR, kI, kA = ("seg", 0, i2), ("seg", 2, i2), ("seg", 1, i2)
                    P.op("act", lambda e: e.activation(out=rr_, in_=rr_, func=AF.Sqrt, bias=1.0, scale=-1.0), writes=[kR])
                    P.op("dve", lambda e: e.scalar_tensor_tensor(out=ii_, in0=ii_, scalar=0.5, in1=rr_, op0=ALU.mult, op1=ALU.mult),
                         reads=[kR], writes=[kI])
                    if d == 0:
                        dst = Hc[:, 0, :] if sid == "c" else Hf[:, sid * 512:(sid + 1) * 512]
                        dkey = ("hcx", 0) if sid == "c" else ("hf", sid)
                        a_ap, b_ap, o_ap = aa_, ii_, dst
                        ncarry = dst[:, N - 1:N]
                    else:
                        dst = Hc[:, 1, :] if sid == "c" else Hb[:, i2, :]
                        dkey = ("hcx", 1) if sid == "c" else ("seg", 3, i2)
                        a_ap, b_ap, o_ap = aa_[:, ::-1], ii_[:, ::-1], dst[:, ::-1]
                        ncarry = dst[:, 0:1]
                    init = 0.0 if state["carry"] is None else state["carry"]
                    ck = [state["ckey"]] if state["ckey"] else []
                    P.op("dve", lambda e: e.tensor_tensor_scan(out=o_ap, data0=a_ap, data1=b_ap, initial=init, op0=ALU.mult, op1=ALU.add),
                         reads=[kA, kI] + ck, writes=[dkey])
                    state["carry"], state["ckey"] = ncarry, dkey
                    if d == 1 and sid != "c":
                        P.op("dve", lambda e: e.tensor_tensor(out=Hf[:, sid * 512:(sid + 1) * 512], in0=Hf[:, sid * 512:(sid + 1) * 512],
                                                               in1=Hb[:, i2, :], op=ALU.add),
                             reads=[("seg", 3, i2)], writes=[("hf", sid)])

                for p0 in range(0, len(order), 2):
                    pair = list(enumerate(order))[p0:p0 + 2]
                    for si, (sid, N, off) in pair:
                        seg_front(si, sid, N, off)
                    for si, (sid, N, off) in pair:
                        seg_back(si, sid, N, off)
            for r in range(NR):
                gb_ = r % 2

                def mmga(e, slot=slot, r=r, gb_=gb_):
                    for k in range(KC):
                        ins = e.matmul(banks[gb_][:], lhsT=wA[slot][:, k, 128:256], rhs=h_cm[:, k, r * 512:(r + 1) * 512],
                                       start=(k == 0), stop=(k == KC - 1))
                    return ins
                P.op("pe", mmga, reads=[("wA", slot, 1)] + hkeys(r), writes=[("b", gb_)])
                P.op("act", lambda e, gb_=gb_: e.activation(out=Gt[:, gb_, :], in_=banks[gb_][:], func=AF.Gelu_apprx_tanh),
                     writes=[("seg", 4, gb_), ("b", gb_)])
                P.op("dve", lambda e, r=r, gb_=gb_, cc=cc: e.tensor_tensor(out=catA[:, cc, r * 512:(r + 1) * 512], in0=Hf[:, r * 512:(r + 1) * 512],
                                                                            in1=Gt[:, gb_, :], op=ALU.mult),
                     reads=[("hf", r), ("seg", 4, gb_)], writes=[("cat", cc, r)])
        barrier_keep(mA)
        phase_out(ab_w_out, lambda k: catA[:, k, :] if k < 4 else catB[:, k - 4, :],
                  extra=(mod_steps(1, False, bb=4, do_barrier=False, nbuf=2, vlist=(0, 1, 2)) if overlap_mod1 else None))

    phase0()
    overlap_mod1 = (tuple(layers) == (0, 1) and "mix" in parts)
    for l in layers:
        CL[0] = l
        if dbg == "p0":
            break
        gen0 = None
        if l == 0 and overlap_mod1:
            gen0 = mod_steps(0, True, bb=5, do_barrier=False, nbuf=3)
            for _ in range(4):
                next(gen0)
        elif not (l == 1 and overlap_mod1):
            phase_mod(l, need_ctx=(l == 0))
        if dbg == "mod":
            break
        if "mix" in parts:
            phase_norm(0, extra=gen0)
            if l == 0:
                phase_mix0()
            else:
                phase_mix1()
            if dbg == "mix":
                break
        if "ffn" in parts:
            phase_norm(1, router=(l == 1),
                       extra=(mod_steps(1, False, bb=5, do_barrier=False, nbuf=3, vlist=(3, 4, 5)) if (l == 0 and overlap_mod1) else None))
            if dbg == "norm":
                break
            if l == 0:
                phase_ffn([(ffn_w1, ffn_w3, ffn_w2, DFF, None)])
            else:
                phase_ffn([(moe_w1[e], moe_w3[e], moe_w2[e], DFFE, e) for e in range(moe_experts)], final=(l == layers[-1]))
    if not out_done[0]:
        for j in range(NT):
            P.op("sp", lambda e, j=j: e.dma_start(out=out_d[j * 128:(j + 1) * 128, :], in_=x_tm[:, j, :]), reads=[("x", j)], dma=True)
    P.emit()
    st.close()
    return nc


def _dft_tables():
    import ml_dtypes
    c = np.arange(128, dtype=np.float64)
    a128 = 2.0 * np.pi * np.outer(c, c) / 128.0
    cs128 = np.concatenate([np.cos(a128), np.sin(a128)], axis=1) / np.sqrt(128.0)
    t = np.arange(T, dtype=np.int64)
    ang = 2.0 * np.pi * ((np.outer(t, t) % T).astype(np.float64)) / T
    CL = np.cos(ang) / np.sqrt(float(T))
    SL = -np.sin(ang) / np.sqrt(float(T))
    both = np.stack([CL, SL], axis=1)
    dft = both.reshape(NT, 128, 2, NR, 512).transpose(3, 0, 1, 2, 4)
    return {"cs128": np.ascontiguousarray(cs128).astype(ml_dtypes.bfloat16),
            "dft": np.ascontiguousarray(dft).astype(ml_dtypes.bfloat16)}


def make_in_maps(inputs):
    f = lambda a: np.ascontiguousarray(np.asarray(a, dtype=np.float32))
    shared = {
        "ident": np.eye(128, dtype=np.float32),
        "ada_w": f(inputs["ada_w"]), "ada_b": f(inputs["ada_b"]), "norm_g": f(inputs["norm_g"]),
        "ffn_w1": f(inputs["ffn_w1"][0]), "ffn_w3": f(inputs["ffn_w3"][0]), "ffn_w2": f(inputs["ffn_w2"][0]),
        "moe_router": f(inputs["moe_router"][0]),
        "moe_w1": f(inputs["moe_w1"][0]), "moe_w3": f(inputs["moe_w3"][0]), "moe_w2": f(inputs["moe_w2"][0]),
        "ab_w_in": f(inputs["ab_w_in"][0]), "rg_conv_w": f(inputs["rg_conv_w"][0]), "rg_conv_b": f(inputs["rg_conv_b"]),
        "rg_w_r": f(inputs["rg_w_r"][0]), "rg_b_r": f(inputs["rg_b_r"][0]), "rg_w_i": f(inputs["rg_w_i"][0]), "rg_b_i": f(inputs["rg_b_i"][0]),
        "rg_lambda": f(inputs["rg_lambda"][0]), "cv_dw_w": f(inputs["cv_dw_w"][0]), "cv_dw_b": f(inputs["cv_dw_b"]),
        "cv_ln_g": f(inputs["cv_ln_g"]), "cv_ln_b": f(inputs["cv_ln_b"]), "ab_w_out": f(inputs["ab_w_out"][0]),
        "cd_w_in": f(inputs["cd_w_in"][0]), "sg_ln_g": f(inputs["sg_ln_g"]), "sg_ln_b": f(inputs["sg_ln_b"]),
        "sg_w_s": f(inputs["sg_w_s"][0]), "sg_b_s": f(inputs["sg_b_s"][0]), "cd_w_out": f(inputs["cd_w_out"][0]),
    }
    shared.update(_dft_tables())
    maps = []
    for b in range(8):
        m = dict(shared)
        m["x"] = f(inputs["x"][b])
        m["ctx"] = f(inputs["ctx"][b])
        m["cc"] = np.concatenate([f(inputs["c"][b]).reshape(8, 128), f(inputs["c_ctx"]).reshape(8, 128)], axis=0)
        maps.append(m)
    return maps


def kernel(**inputs):
    nc = bass.Bass("TRN2", target_bir_lowering=False)
    build(nc)
    maps = make_in_maps(inputs)
    res = run_bass_kernel_spmd(nc, maps, core_ids=list(range(8)))
    return np.stack([np.asarray(r["out"], dtype=np.float32) for r in res.results], axis=0)
```

```python
import contextlib
import numpy as np
import concourse.bass as bass
import concourse.mybir as mybir
from concourse.bass_utils import run_bass_kernel_spmd

F32 = mybir.dt.float32
BF16 = mybir.dt.bfloat16
AF = mybir.ActivationFunctionType
ALU = mybir.AluOpType

D = 1024
T = 2048
CTX = 256
NT = T // 128
NR = T // 512
KC = D // 128
DFF = 2816
DFFE = 3584
NE = 8
FG = 256
NORM_EPS = 1e-6
LN_EPS = 1e-5
ENGS = ("pe", "act", "dve", "pool", "sp")


class _Op:
    __slots__ = ("eng", "fn", "reads", "writes", "dma", "deps", "sig", "needs_sig", "idx")

    def __init__(self, eng, fn, reads, writes, dma):
        self.eng, self.fn, self.reads, self.writes, self.dma = eng, fn, reads, writes, dma
        self.deps = []
        self.sig = None
        self.needs_sig = False


class Prog:
    DMA_POOL = 8

    def __init__(self, nc):
        self.nc = nc
        self.ops = []

    def op(self, eng, fn, reads=(), writes=(), dma=False, phase=True):
        rd = tuple(reads) + (("PHASE",) if phase else ())
        o = _Op(eng, fn, rd, tuple(writes), dma)
        o.needs_sig = dma
        o.idx = len(self.ops)
        self.ops.append(o)
        return o

    def _analyse(self):
        last_w, readers = {}, {}
        dma_hist = {e: [] for e in ENGS}
        ops = self.ops
        for o in ops:
            deps = set()
            for r in o.reads:
                w = last_w.get(r)
                if w is not None:
                    deps.add(w)
            for w in o.writes:
                lw = last_w.get(w)
                if lw is not None:
                    deps.add(lw)
                rl = readers.get(w)
                if rl:
                    deps.update(rl)
            if o.dma:
                h = dma_hist[o.eng]
                if len(h) >= self.DMA_POOL:
                    deps.add(h[-self.DMA_POOL])
                h.append(o.idx)
            deps.discard(o.idx)
            if o.eng == "pe":
                deps = {d for d in deps if ops[d].eng != "pe"}
            o.deps = deps
            for d in deps:
                ops[d].needs_sig = True
            for r in o.reads:
                readers.setdefault(r, []).append(o.idx)
            for w in o.writes:
                last_w[w] = o.idx
                readers[w] = []

    def emit(self):
        nc = self.nc
        self._analyse()
        stack = contextlib.ExitStack()
        csem = {e: stack.enter_context(nc.semaphore("c_" + e)) for e in ("pe", "act", "dve", "pool")}
        dsem = {e: [stack.enter_context(nc.semaphore("d_%s_%d" % (e, j))) for j in range(self.DMA_POOL)]
                for e in ("sp", "pool", "act")}
        ccount = {e: 0 for e in csem}
        dcount = {e: 0 for e in dsem}
        for o in self.ops:
            if not o.needs_sig:
                continue
            if o.dma:
                n = dcount[o.eng]
                dcount[o.eng] += 1
                o.sig = (dsem[o.eng][n % self.DMA_POOL], 16 * (n // self.DMA_POOL + 1), 16)
            else:
                ccount[o.eng] += 1
                o.sig = (csem[o.eng], ccount[o.eng], 1)
        per_eng = {e: [o for o in self.ops if o.eng == e] for e in ENGS}
        ops = self.ops
        K = self.DMA_POOL

        def run(engname, engobj):
            seen = {}
            for o in per_eng[engname]:
                need = {}
                for d in o.deps:
                    sem, val, _ = ops[d].sig
                    k = id(sem)
                    if seen.get(k, 0) >= val:
                        continue
                    if k not in need or need[k][1] < val:
                        need[k] = (sem, val)
                for k, (sem, val) in need.items():
                    engobj.wait_ge(sem, val)
                    seen[k] = val
                ins = o.fn(engobj)
                if o.needs_sig:
                    ins.then_inc(o.sig[0], o.sig[2])
            if engname == "sp":
                for e in dsem:
                    n = dcount[e]
                    for j in range(min(n, K)):
                        engobj.wait_ge(dsem[e][j], 16 * ((n - 1 - j) // K + 1))

        with nc.Block() as block:
            @block.tensor
            def _(e):
                run("pe", e)

            @block.scalar
            def _(e):
                run("act", e)

            @block.vector
            def _(e):
                run("dve", e)

            @block.gpsimd
            def _(e):
                run("pool", e)

            @block.sync
            def _(e):
                run("sp", e)
        stack.close()


class Region:
    def __init__(self, tensor, nwords):
        self.t = tensor
        self.n = nwords
        self.off = 0

    def reset(self):
        self.off = 0

    def mark(self):
        return self.off

    def release(self, m):
        self.off = m

    def alloc(self, shape, dt=F32):
        assert shape[0] == 128
        nel = int(np.prod(shape[1:]))
        words = nel if dt == F32 else (nel + 1) // 2
        assert self.off + words <= self.n, "region overflow: need %d words at %d of %d" % (words, self.off, self.n)
        v = self.t[:, self.off:self.off + words]
        self.off += words
        if dt != F32:
            v = v.bitcast(dt)[:, 0:nel]
        if len(shape) == 3:
            v = v.rearrange("p (a b) -> p a b", b=shape[2])
        elif len(shape) == 4:
            v = v.rearrange("p (a b c) -> p a b c", b=shape[2], c=shape[3])
        return v


def build(nc, layers=(0, 1), parts=("mix", "ffn"), moe_experts=NE, dbg=None):
    st = contextlib.ExitStack()

    def din(name, shape, dt=F32):
        return nc.dram_tensor(name, list(shape), dt, kind="ExternalInput").ap()

    x_d = din("x", [T, D])
    ctx_d = din("ctx", [CTX, D])
    cc_d = din("cc", [16, 128])
    ident_d = din("ident", [128, 128])
    ada_w = din("ada_w", [2, D, 6 * D])
    ada_b = din("ada_b", [2, 6 * D])
    norm_g = din("norm_g", [2, 4, D])
    ffn_w1 = din("ffn_w1", [D, DFF])
    ffn_w3 = din("ffn_w3", [D, DFF])
    ffn_w2 = din("ffn_w2", [DFF, D])
    moe_router = din("moe_router", [D, NE])
    moe_w1 = din("moe_w1", [NE, D, DFFE])
    moe_w3 = din("moe_w3", [NE, D, DFFE])
    moe_w2 = din("moe_w2", [NE, DFFE, D])
    ab_w_in = din("ab_w_in", [D, 2048])
    rg_conv_w = din("rg_conv_w", [4, 512])
    rg_conv_b = din("rg_conv_b", [1, 512])
    rg_w_r = din("rg_w_r", [2, 8, 64, 64])
    rg_b_r = din("rg_b_r", [2, 512])
    rg_w_i = din("rg_w_i", [2, 8, 64, 64])
    rg_b_i = din("rg_b_i", [2, 512])
    rg_lambda = din("rg_lambda", [2, 512])
    cv_dw_w = din("cv_dw_w", [31, 512])
    cv_dw_b = din("cv_dw_b", [1, 512])
    cv_ln_g = din("cv_ln_g", [1, 512])
    cv_ln_b = din("cv_ln_b", [1, 512])
    ab_w_out = din("ab_w_out", [D, D])
    cd_w_in = din("cd_w_in", [D, 1536])
    sg_ln_g = din("sg_ln_g", [1, 512])
    sg_ln_b = din("sg_ln_b", [1, 512])
    sg_w_s = din("sg_w_s", [8, 128, 128])
    sg_b_s = din("sg_b_s", [8, 128])
    cd_w_out = din("cd_w_out", [D, D])
    cs128_d = din("cs128", [128, 256], BF16)
    dft_d = din("dft", [NR, NT, 128, 2, 512], BF16)
    out_d = nc.dram_tensor("out", [T, D], F32, kind="ExternalOutput").ap()

    def sb(name, shape, dt=F32):
        return st.enter_context(nc.sbuf_tensor("sb_" + name, list(shape), dt))

    x_tm = sb("x_tm_sb", [128, NT, D])
    h_cm = sb("h_cm_sb", [128, KC, T], BF16)
    cols = sb("cols", [128, 384])
    ident = sb("ident_sb", [128, 128])
    gg_all = sb("gg_bc", [128, 2, 2, D], BF16)
    modc_all = sb("modc", [128, 2, 8, KC])
    ggtmp_holder = {}
    gates = sb("gates", [128, NT, NE])
    stat = sb("stat", [128, 64])
    dummy = sb("dummy", [128, 2])
    REG_WORDS = 25600
    reg_t = sb("region", [128, REG_WORDS])
    R = Region(reg_t, REG_WORDS)
    banks = [st.enter_context(nc.psum_tensor("bank%d" % i, [128, 512], F32)) for i in range(8)]

    P = Prog(nc)

    def barrier():
        P.op("pool", lambda e: e.memset(dummy[:, 0:1], 0.0), writes=["PHASE"], phase=False)
        R.reset()

    colmap = {}
    rows_used = [0]

    def phase0():
        rows = R.alloc([128, 3, 128])
        io_ = R.alloc([128, 256])
        om_ = R.alloc([128, 256])
        pv_ = R.alloc([128, 4])
        ang_ = R.alloc([128, 2, 256])
        tt_ = R.alloc([128, 2, 256])
        tab_ = R.alloc([128, 2, 512])
        s2_ = R.alloc([128, 2, 256])
        sel_ = R.alloc([128, 16, 128])[0:32]
        zer_ = R.alloc([128, 16, 128])[0:32]
        MAGIC = 12582912.0
        TWO_PI = 6.283185307179586
        P.op("pool", lambda e: e.iota(io_[:, :], [[1, 256]], base=0, channel_multiplier=0, allow_small_or_imprecise_dtypes=True), writes=["pe_io"])
        P.op("pool", lambda e: e.iota(pv_[0:32, 0:1], [[0, 1]], base=0, channel_multiplier=1, allow_small_or_imprecise_dtypes=True), writes=["pe_pv0"])
        P.op("pool", lambda e: e.iota(pv_[0:64, 1:2], [[0, 1]], base=0, channel_multiplier=1, allow_small_or_imprecise_dtypes=True), writes=["pe_pv1a"])
        P.op("pool", lambda e: e.iota(pv_[64:128, 1:2], [[0, 1]], base=0, channel_multiplier=1, allow_small_or_imprecise_dtypes=True), writes=["pe_pv1b"])
        P.op("pool", lambda e: e.iota(sel_.rearrange("k j (h q) -> k j h q", h=2), [[2, 16], [1, 2], [0, 64]], base=0, channel_multiplier=-1,
                                      allow_small_or_imprecise_dtypes=True), writes=["pe_sel0"])
        P.op("dve", lambda e: e.memset(zer_, 0.0), writes=["pe_zer"])
        P.op("dve", lambda e: e.tensor_tensor(out=sel_, in0=sel_, in1=zer_, op=ALU.is_equal), reads=["pe_sel0", "pe_zer"], writes=["pe_sel"])
        P.op("act", lambda e: e.activation(out=om_[:, :], in_=io_[:, :], func=AF.Exp, scale=-9.210340371976184 / 256.0), reads=["pe_io"], writes=["pe_om"])
        P.op("dve", lambda e: e.tensor_scalar(out=ang_[0:32, 0, :], in0=om_[0:32, :], scalar1=pv_[0:32, 0:1], scalar2=None, op0=ALU.mult),
             reads=["pe_om", "pe_pv0"], writes=["pe_ang0"])
        P.op("dve", lambda e: e.tensor_scalar(out=ang_[:, 1, :], in0=om_[:, :], scalar1=pv_[:, 1:2], scalar2=None, op0=ALU.mult),
             reads=["pe_om", "pe_pv1a", "pe_pv1b"], writes=["pe_ang1"])
        for i_, np_ in ((0, 32), (1, 128)):
            a_ = ang_[0:np_, i_, :]
            t_ = tt_[0:np_, i_, :]
            q_ = s2_[0:np_, i_, :]
            P.op("dve", lambda e, a_=a_, t_=t_: e.tensor_scalar(out=t_, in0=a_, scalar1=1.0 / TWO_PI, scalar2=MAGIC, op0=ALU.mult, op1=ALU.add),
                 reads=["pe_ang%d" % i_], writes=["pe_t%d" % i_])
            P.op("dve", lambda e, t_=t_: e.tensor_scalar(out=t_, in0=t_, scalar1=MAGIC, scalar2=-TWO_PI, op0=ALU.subtract, op1=ALU.mult), writes=["pe_t%d" % i_])
            P.op("dve", lambda e, a_=a_, t_=t_: e.tensor_tensor(out=t_, in0=t_, in1=a_, op=ALU.add), reads=["pe_ang%d" % i_], writes=["pe_t%d" % i_])
            P.op("dve", lambda e, t_=t_: e.tensor_scalar(out=t_, in0=t_, scalar1=-3.1415925, scalar2=3.1415925, op0=ALU.max, op1=ALU.min), writes=["pe_t%d" % i_])
            P.op("act", lambda e, t_=t_, i_=i_, np_=np_: e.activation(out=tab_[0:np_, i_, 0:256], in_=t_, func=AF.Sin), reads=["pe_t%d" % i_], writes=["pe_sin%d" % i_])
            P.op("act", lambda e, t_=t_, q_=q_: e.activation(out=q_, in_=t_, func=AF.Sin, scale=0.5), reads=["pe_t%d" % i_], writes=["pe_q%d" % i_])
            P.op("dve", lambda e, q_=q_: e.tensor_tensor(out=q_, in0=q_, in1=q_, op=ALU.mult), writes=["pe_q%d" % i_])
            P.op("dve", lambda e, q_=q_, i_=i_, np_=np_: e.tensor_scalar(out=tab_[0:np_, i_, 256:512], in0=q_, scalar1=-2.0, scalar2=1.0, op0=ALU.mult, op1=ALU.add),
                 reads=["pe_q%d" % i_], writes=["pe_cos%d" % i_])
        P.op("dve", lambda e: e.memset(rows[:, :, :], 0.0), writes=["rows"])
        P.op("sp", lambda e: e.dma_start(out=ident[:], in_=ident_d), writes=["ident"], dma=True)

        def load_rows(name, ap2d):
            n = ap2d.shape[0]
            r0 = rows_used[0]
            if (r0 % 128) + n > 128:
                r0 = (r0 // 128 + 1) * 128
            colmap[name] = r0
            rows_used[0] = r0 + n
            assert rows_used[0] <= 384
            P.op("sp", lambda e: e.dma_start(out=rows[r0 % 128:r0 % 128 + n, r0 // 128, :], in_=ap2d),
                 reads=["rows"], writes=[("rowsd", name)], dma=True)

        load_rows("cc", cc_d)
        for l in range(2):
            load_rows("normg%d" % l, norm_g[l].rearrange("a (c p) -> (a c) p", p=128))
            load_rows("adab%d" % l, ada_b[l:l + 1, :].rearrange("a (c p) -> (a c) p", p=128))
        extra_rows(load_rows)
        names = list(colmap.keys())
        for i in range(3):
            P.op("pe", lambda e, i=i: e.transpose(out=banks[0][:, i * 128:(i + 1) * 128], in_=rows[:, i, :], identity=ident[:]),
                 reads=[("rowsd", n) for n in names] + ["ident", "rows"], writes=[("b", 0)])
        P.op("dve", lambda e: e.tensor_copy(out=cols[:], in_=banks[0][:, 0:384]), writes=["cols", ("b", 0)])
        for j in range(NT):
            P.op("sp", lambda e, j=j: e.dma_start(out=x_tm[:, j, :], in_=x_d[j * 128:(j + 1) * 128, :]),
                 writes=[("x", j)], dma=True)
            pb_ = 1 + (j % 2)
            P.op("pe", lambda e, j=j, pb_=pb_: e.matmul(banks[pb_][:], lhsT=sel_[:, j, :], rhs=tab_[0:32, 0, :], start=True, stop=True),
                 reads=["pe_sel", "pe_sin0", "pe_cos0"], writes=[("b", pb_)])
            P.op("dve", lambda e, j=j, pb_=pb_: e.tensor_tensor(out=x_tm[:, j, 0:512], in0=x_tm[:, j, 0:512], in1=banks[pb_][:], op=ALU.add),
                 reads=[("x", j)], writes=[("x", j), ("b", pb_)])
            P.op("dve", lambda e, j=j: e.tensor_tensor(out=x_tm[:, j, 512:1024], in0=x_tm[:, j, 512:1024], in1=tab_[:, 1, :], op=ALU.add),
                 reads=[("x", j), "pe_sin1", "pe_cos1"], writes=[("x", j)])
        barrier()

    def extra_rows(load_rows):
        r2 = lambda ap: ap.rearrange("a (c p) -> (a c) p", p=128)
        load_rows("rg_conv_w", r2(rg_conv_w))
        load_rows("rg_conv_b", r2(rg_conv_b))
        load_rows("rg_b_r", r2(rg_b_r))
        load_rows("rg_b_i", r2(rg_b_i))
        load_rows("rg_lambda", r2(rg_lambda))
        load_rows("cv_dw_b", r2(cv_dw_b))
        load_rows("cv_ln_g", r2(cv_ln_g))
        load_rows("cv_ln_b", r2(cv_ln_b))
        load_rows("cv_dw_w", r2(cv_dw_w))

    def mod_steps(l, need_ctx, bb=1, do_barrier=True, nbuf=3, vlist=(0, 1, 2, 3, 4, 5)):
        gg_bc = gg_all[:, l]
        modc = modc_all[:, l]
        ggf = R.alloc([128, 2, 512])
        wa = [R.alloc([128, KC, 512], BF16) for _ in range(nbuf)]
        bc = R.alloc([128, 4, D])
        silu_cc = R.alloc([128, KC, 2], BF16)
        silu_rep = R.alloc([128, KC, 128], BF16)
        mraw = R.alloc([128, 4, KC, 2])
        c0 = colmap["cc"]
        P.op("act", lambda e: e.activation(out=silu_cc[:, :, 0], in_=cols[:, c0:c0 + 8], func=AF.Silu),
             reads=["cols"], writes=["silu0"])
        P.op("act", lambda e: e.activation(out=silu_cc[:, :, 1], in_=cols[:, c0 + 8:c0 + 16], func=AF.Silu),
             reads=["cols"], writes=["silu1"])
        for k in range(KC):
            P.op("dve", lambda e, k=k: e.tensor_copy(out=silu_rep[:, k, :], in_=silu_cc[:, k, 0:1].to_broadcast([128, 128])),
                 reads=["silu0"], writes=[("silurep", k)])
        srcs = [ada_b[l:l + 1, 2 * D:3 * D], ada_b[l:l + 1, 5 * D:6 * D], norm_g[l, 1:2, :], norm_g[l, 3:4, :]]
        for i, s in enumerate(srcs):
            P.op("sp", lambda e, i=i, s=s: e.dma_start(out=bc[:, i, :], in_=s.to_broadcast([128, D])),
                 writes=[("bc", i)], dma=True)
        nload = [0]
        ab0 = colmap["adab%d" % l]
        ng0 = colmap["normg%d" % l]
        def derive(dst, sci, shi, gcol, who):
            P.op("dve", lambda e: e.scalar_tensor_tensor(
                out=modc[:, dst, :], in0=mraw[:, sci, :, who], scalar=1.0, in1=cols[:, gcol:gcol + 8], op0=ALU.add, op1=ALU.mult),
                reads=[("mraw", sci), "cols"], writes=[("modc", l, dst)])
            P.op("dve", lambda e: e.tensor_copy(out=modc[:, dst + 1, :], in_=mraw[:, shi, :, who]),
                 reads=[("mraw", shi)], writes=[("modc", l, dst + 1)])

        for v in vlist:
            for half in range(2):
                n = nload[0]
                nload[0] += 1
                wb = wa[n % nbuf]
                src = ada_w[l][:, v * D + half * 512: v * D + (half + 1) * 512].rearrange("(k p) f -> p k f", p=128)
                P.op("pool", lambda e, wb=wb, src=src: e.dma_start(out=wb[:], in_=src), writes=[("wa", n % nbuf)], dma=True)
                if v in (2, 5):
                    gi = 0 if v == 2 else 1
                    bk = bb + (n % 2)

                    def mm(e, wb=wb, bk=bk):
                        for k in range(KC):
                            ins = e.matmul(banks[bk][:], lhsT=silu_rep[:, k, :], rhs=wb[:, k, :], start=(k == 0), stop=(k == KC - 1))
                        return ins
                    P.op("pe", mm, reads=[("wa", n % nbuf)] + [("silurep", k) for k in range(KC)], writes=[("b", bk)])
                    sl = slice(half * 512, (half + 1) * 512)
                    gt = ggf[:, n % 2, :]
                    P.op("dve", lambda e, bk=bk, gi=gi, sl=sl, gt=gt: e.tensor_tensor(out=gt, in0=banks[bk][:], in1=bc[:, gi, sl], op=ALU.add),
                         reads=[("bc", gi)], writes=[("ggf", n % 2), ("b", bk)])
                    P.op("dve", lambda e, gi=gi, sl=sl, gt=gt: e.tensor_tensor(out=gg_bc[:, gi, sl], in0=gt, in1=bc[:, 2 + gi, sl], op=ALU.mult),
                         reads=[("ggf", n % 2), ("bc", 2 + gi)], writes=[("gg", l, gi, half)])
                else:
                    vi = {0: 0, 1: 1, 3: 2, 4: 3}[v]

                    def mm(e, wb=wb, half=half):
                        for oc in range(4):
                            for k in range(KC):
                                ins = e.matmul(banks[bb + 2][:, (half * 4 + oc) * 2:(half * 4 + oc) * 2 + 2], lhsT=wb[:, k, oc * 128:(oc + 1) * 128],
                                               rhs=silu_cc[:, k, :], start=(k == 0), stop=(k == KC - 1))
                        return ins
                    P.op("pe", mm, reads=[("wa", n % nbuf), "silu0", "silu1"], writes=[("b", bb + 2)])
                    if half == 1:
                        a0 = ab0 + v * 8
                        P.op("dve", lambda e, vi=vi, a0=a0: e.tensor_tensor(
                            out=mraw[:, vi, :, :], in0=banks[bb + 2][:, 0:16].rearrange("p (a b) -> p a b", b=2),
                            in1=cols[:, a0:a0 + 8].unsqueeze(2).to_broadcast([128, 8, 2]), op=ALU.add),
                            reads=["cols"], writes=[("mraw", vi), ("b", bb + 2)])
                        if v == 1:
                            derive(0, 1, 0, ng0 + 0, 0)
                            if need_ctx:
                                derive(2, 1, 0, ng0 + 0, 1)
                        if v == 4:
                            derive(4, 3, 2, ng0 + 16, 0)
                yield
        if do_barrier:
            barrier()
        yield

    def phase_mod(l, need_ctx):
        for _ in mod_steps(l, need_ctx):
            pass

    def phase_norm(which, router=False, extra=None):
        sc_i, sh_i = (0, 1) if which == 0 else (4, 5)
        L = CL[0]
        modc = modc_all[:, L]
        junk = R.alloc([128, D], BF16)
        xs = R.alloc([128, 2, 4, D])
        ss = stat[:, 0:16]
        rstd = stat[:, 16:32]
        if router:
            h32 = R.alloc([128, 2, KC, 512])
            rt = R.alloc([128, KC, NE])
            lg = R.alloc([128, NT, NE])
            m8 = R.alloc([128, NT, 8])
            ex = R.alloc([128, NT, NE])
            P.op("sp", lambda e: e.dma_start(out=rt[:], in_=moe_router.rearrange("(k p) n -> p k n", p=128)), writes=["rt"], dma=True)
        P.op("dve", lambda e: e.memset(ss, 0.0), writes=["ss"])

        def stage0(r):
            for j in range(4 * r, 4 * r + 4):
                P.op("act", lambda e, j=j: e.activation(out=junk[:], in_=x_tm[:, j, :], func=AF.Square, accum_out=ss[:, j:j + 1]),
                     reads=[("x", j), "ss"], writes=[("ssj", j), "junk"])
            sl = slice(4 * r, 4 * r + 4)
            P.op("act", lambda e, sl=sl: e.activation(out=rstd[:, sl], in_=ss[:, sl], func=AF.Sqrt, bias=NORM_EPS, scale=1.0 / D),
                 reads=[("ssj", j) for j in range(4 * r, 4 * r + 4)], writes=[("rstd0", r)])
            P.op("dve", lambda e, sl=sl: e.reciprocal(out=rstd[:, sl], in_=rstd[:, sl]), reads=[("rstd0", r)], writes=[("rstd", r)])

        def stage1(r):
            for jj in range(4):
                j = 4 * r + jj
                P.op("dve", lambda e, j=j, jj=jj, r=r: e.tensor_scalar(out=xs[:, r % 2, jj, :], in0=x_tm[:, j, :], scalar1=rstd[:, j:j + 1], scalar2=None,
                                                                        op0=ALU.mult),
                     reads=[("x", j), ("rstd", r)], writes=[("xs", r % 2, jj)])

        def stage2(r):
            rb = r % 2
            for k in range(KC):
                u = r * KC + k
                bk = u % (5 if extra is not None else 6)

                def tr(e, rb=rb, k=k, bk=bk):
                    for jj in range(4):
                        ins = e.transpose(out=banks[bk][:, jj * 128:(jj + 1) * 128], in_=xs[:, rb, jj, k * 128:(k + 1) * 128], identity=ident[:])
                    return ins
                P.op("pe", tr, reads=[("xs", rb, jj) for jj in range(4)] + ["ident"], writes=[("b", bk)])
                hk = [("h", k, j) for j in range(4 * r, 4 * r + 4)]
                dst = h_cm[:, k, r * 512:(r + 1) * 512]
                if u % 2 == 0:
                    P.op("dve", lambda e, k=k, bk=bk, dst=dst: e.tensor_scalar(out=dst, in0=banks[bk][:], scalar1=modc[:, sc_i, k:k + 1],
                                                                                scalar2=modc[:, sh_i, k:k + 1], op0=ALU.mult, op1=ALU.add),
                         reads=[("modc", L, sc_i), ("modc", L, sh_i)], writes=hk + [("b", bk)])
                    if router:
                        P.op("dve", lambda e, k=k, bk=bk, rb=rb: e.tensor_scalar(out=h32[:, rb, k, :], in0=banks[bk][:], scalar1=modc[:, sc_i, k:k + 1],
                                                                                  scalar2=modc[:, sh_i, k:k + 1], op0=ALU.mult, op1=ALU.add),
                             reads=[("modc", L, sc_i), ("modc", L, sh_i)], writes=[("h32", rb, k), ("b", bk)])
                else:
                    P.op("act", lambda e, k=k, bk=bk, dst=dst: e.activation(out=dst, in_=banks[bk][:], func=AF.Identity, bias=modc[:, sh_i, k:k + 1],
                                                                             scale=modc[:, sc_i, k:k + 1]),
                         reads=[("modc", L, sc_i), ("modc", L, sh_i)], writes=hk + [("b", bk)])
                    if router:
                        P.op("act", lambda e, k=k, bk=bk, rb=rb: e.activation(out=h32[:, rb, k, :], in_=banks[bk][:], func=AF.Identity,
                                                                               bias=modc[:, sh_i, k:k + 1], scale=modc[:, sc_i, k:k + 1]),
                             reads=[("modc", L, sc_i), ("modc", L, sh_i)], writes=[("h32", rb, k), ("b", bk)])
                if extra is not None and u % 4 == 3:
                    next(extra, None)
            if router:
                for jj in range(4):
                    j = 4 * r + jj
                    lb = 6 + (j % 2)

                    def rmm(e, rb=rb, jj=jj, lb=lb):
                        for k in range(KC):
                            ins = e.matmul(banks[lb][:, 0:NE], lhsT=h32[:, rb, k, jj * 128:(jj + 1) * 128], rhs=rt[:, k, :], start=(k == 0), stop=(k == KC - 1))
                        return ins
                    P.op("pe", rmm, reads=[("h32", rb, k) for k in range(KC)] + ["rt"], writes=[("b", lb)])
                    P.op("dve", lambda e, j=j, lb=lb: e.tensor_copy(out=lg[:, j, :], in_=banks[lb][:, 0:NE]), writes=[("lg", j), ("b", lb)])
                    P.op("dve", lambda e, j=j: e.max(out=m8[:, j, :], in_=lg[:, j, :]), reads=[("lg", j)], writes=[("m8", j)])

        stage0(0)
        stage0(1)
        stage1(0)
        for r in range(NR):
            if r + 2 < NR:
                stage0(r + 2)
            if r + 1 < NR:
                stage1(r + 1)
            stage2(r)
        if extra is not None:
            for _ in extra:
                pass
        if router:
            allj = list(range(NT))
            P.op("dve", lambda e: e.tensor_tensor(out=ex[:], in0=lg[:], in1=m8[:, :, 0:1].to_broadcast([128, NT, NE]), op=ALU.subtract),
                 reads=[("lg", j) for j in allj] + [("m8", j) for j in allj], writes=["ex0"])
            P.op("act", lambda e: e.activation(out=ex[:], in_=ex[:], func=AF.Exp), reads=["ex0"], writes=["ex1"])
            P.op("dve", lambda e: e.tensor_tensor(out=lg[:], in0=lg[:], in1=m8[:, :, 1:2].to_broadcast([128, NT, NE]), op=ALU.is_ge),
                 reads=["ex0"], writes=["mask"])
            P.op("dve", lambda e: e.tensor_tensor(out=ex[:], in0=ex[:], in1=lg[:], op=ALU.mult), reads=["ex1", "mask"], writes=["ex2"])
            den = stat[:, 32:48]
            P.op("dve", lambda e: e.tensor_reduce(out=den, in_=ex[:], axis=mybir.AxisListType.X, op=ALU.add), reads=["ex2"], writes=["den0"])
            P.op("dve", lambda e: e.reciprocal(out=den, in_=den), reads=["den0"], writes=["den"])
            P.op("dve", lambda e: e.tensor_tensor(out=gates[:], in0=ex[:], in1=den.unsqueeze(2).to_broadcast([128, NT, NE]), op=ALU.mult),
                 reads=["ex2", "den"], writes=["gates"])
        barrier()

    def phase_ffn(experts, final=False):
        L = CL[0]
        gg_bc = gg_all[:, L]
        y_acc = R.alloc([128, NT, D])
        P.op("dve", lambda e: e.memset(stat[:, 0:64], 0.0), writes=["ss"])
        act = R.alloc([128, 2, T], BF16)
        w1b = [R.alloc([128, KC, FG], BF16) for _ in range(2)]
        w3b = [R.alloc([128, KC, FG], BF16) for _ in range(2)]
        w2b = [R.alloc([128, 2, D], BF16) for _ in range(2)]
        sg = [R.alloc([128, 512], BF16) for _ in range(2)]
        groups = [(w1, w3, w2, g, gi) for (w1, w3, w2, F, gi) in experts for g in range(F // FG)]
        NG = len(groups)
        cntA = [0]
        cntB = [0]

        def load(i):
            w1, w3, w2, g, gi = groups[i]
            s = i % 2
            P.op("pool", lambda e: e.dma_start(out=w1b[s][:], in_=w1[:, g * FG:(g + 1) * FG].rearrange("(k p) f -> p k f", p=128)),
                 writes=[("w1b", s)], dma=True)
            P.op("pool", lambda e: e.dma_start(out=w3b[s][:], in_=w3[:, g * FG:(g + 1) * FG].rearrange("(k p) f -> p k f", p=128)),
                 writes=[("w3b", s)], dma=True)
            P.op("pool", lambda e: e.dma_start(out=w2b[s][:], in_=w2[g * FG:(g + 1) * FG, :].rearrange("(c p) d -> p c d", p=128)),
                 writes=[("w2b", s)], dma=True)

        def A_unit(i, r, c):
            s = i % 2
            n = cntA[0]
            cntA[0] += 1
            pb = 2 * (n % 2)
            sgi = n % 2

            def mmA(e):
                for k in range(KC):
                    e.matmul(banks[pb][:], lhsT=w1b[s][:, k, c * 128:(c + 1) * 128], rhs=h_cm[:, k, r * 512:(r + 1) * 512],
                             start=(k == 0), stop=(k == KC - 1))
                for k in range(KC):
                    ins = e.matmul(banks[pb + 1][:], lhsT=w3b[s][:, k, c * 128:(c + 1) * 128], rhs=h_cm[:, k, r * 512:(r + 1) * 512],
                                   start=(k == 0), stop=(k == KC - 1))
                return ins
            P.op("pe", mmA, reads=[("w1b", s), ("w3b", s)] + hkeys(r), writes=[("b", pb), ("b", pb + 1)])
            P.op("act", lambda e: e.activation(out=sg[sgi][:], in_=banks[pb][:], func=AF.Silu), writes=[("sg", sgi), ("b", pb)])
            P.op("dve", lambda e: e.tensor_tensor(out=act[:, c, r * 512:(r + 1) * 512], in0=sg[sgi][:], in1=banks[pb + 1][:], op=ALU.mult),
                 reads=[("sg", sgi)], writes=[("act", c, r), ("b", pb + 1)])

        def B_unit(i, t, dh):
            w1, w3, w2, g, gi = groups[i]
            s = i % 2
            n = cntB[0]
            cntB[0] += 1
            yb = 4 + (n % 4)

            def mmB(e):
                for c in range(2):
                    ins = e.matmul(banks[yb][:], lhsT=act[:, c, t * 128:(t + 1) * 128], rhs=w2b[s][:, c, dh * 512:(dh + 1) * 512],
                                   start=(c == 0), stop=(c == 1))
                return ins
            P.op("pe", mmB, reads=[("act", 0, t // 4), ("act", 1, t // 4), ("w2b", s)], writes=[("b", yb)])
            ya = y_acc[:, t, dh * 512:(dh + 1) * 512]
            first = (i == 0)
            if gi is None:
                if first:
                    P.op("dve", lambda e: e.tensor_copy(out=ya, in_=banks[yb][:]), writes=[("y", t, dh), ("b", yb)])
                else:
                    P.op("dve", lambda e: e.tensor_tensor(out=ya, in0=banks[yb][:], in1=ya, op=ALU.add),
                         reads=[("y", t, dh)], writes=[("y", t, dh), ("b", yb)])
            else:
                gsc = gates[:, t, gi:gi + 1]
                if first:
                    P.op("dve", lambda e: e.tensor_scalar(out=ya, in0=banks[yb][:], scalar1=gsc, scalar2=None, op0=ALU.mult),
                         reads=["gates"], writes=[("y", t, dh), ("b", yb)])
                else:
                    P.op("dve", lambda e: e.scalar_tensor_tensor(out=ya, in0=banks[yb][:], scalar=gsc, in1=ya, op0=ALU.mult, op1=ALU.add),
                         reads=[("y", t, dh), "gates"], writes=[("y", t, dh), ("b", yb)])

        load(0)
        for r in range(NR):
            for c in range(2):
                A_unit(0, r, c)
        for i in range(NG):
            if i + 1 < NG:
                load(i + 1)
            for t in range(4):
                for dh in range(2):
                    B_unit(i, t, dh)
            for r in range(NR):
                for c in range(2):
                    if i + 1 < NG:
                        A_unit(i + 1, r, c)
                    if r + 1 < NR:
                        for t in (4 * (r + 1) + 2 * c, 4 * (r + 1) + 2 * c + 1):
                            for dh in range(2):
                                B_unit(i, t, dh)
        junk = sg[0]
        ss = stat[:, 0:16]
        rstd = stat[:, 16:32]
        def sq(g):
            for t in range(4 * g, 4 * g + 4):
                for dh in range(2):
                    P.op("act", lambda e, t=t, dh=dh: e.activation(out=junk[:], in_=y_acc[:, t, dh * 512:(dh + 1) * 512], func=AF.Square,
                                                                    accum_out=stat[:, 32 + 2 * t + dh:33 + 2 * t + dh]),
                         reads=[("y", t, dh), "ss"], writes=[("ssy", t, dh), ("sg", 0)])

        def fin(g):
            sl = slice(4 * g, 4 * g + 4)
            P.op("dve", lambda e: e.tensor_reduce(out=ss[:, sl], in_=stat[:, 32 + 8 * g:40 + 8 * g].rearrange("p (a b) -> p a b", b=2),
                                                  axis=mybir.AxisListType.X, op=ALU.add),
                 reads=[("ssy", t, dh) for t in range(4 * g, 4 * g + 4) for dh in range(2)] + ["ss"], writes=[("ss2", g)])
            P.op("act", lambda e: e.activation(out=rstd[:, sl], in_=ss[:, sl], func=AF.Sqrt, bias=NORM_EPS, scale=1.0 / D),
                 reads=[("ss2", g)], writes=[("rstd0", g)])
            P.op("dve", lambda e: e.reciprocal(out=rstd[:, sl], in_=rstd[:, sl]), reads=[("rstd0", g)], writes=[("rstdg", g)])

        def upd(g):
            for t in range(4 * g, 4 * g + 4):
                P.op("dve", lambda e, t=t: e.scalar_tensor_tensor(out=y_acc[:, t, :], in0=y_acc[:, t, :], scalar=rstd[:, t:t + 1], in1=gg_bc[:, 1, :],
                                                                   op0=ALU.mult, op1=ALU.mult),
                     reads=[("y", t, 0), ("y", t, 1), ("rstdg", g), ("gg", L, 1, 0), ("gg", L, 1, 1)], writes=[("y", t, 0), ("y", t, 1)])
                P.op("dve" if t % 4 else "pool", lambda e, t=t: e.tensor_tensor(out=x_tm[:, t, :], in0=x_tm[:, t, :], in1=y_acc[:, t, :], op=ALU.add),
                     reads=[("y", t, 0), ("y", t, 1), ("x", t)], writes=[("x", t)])
                if final:
                    P.op("sp", lambda e, t=t: e.dma_start(out=out_d[t * 128:(t + 1) * 128, :], in_=x_tm[:, t, :]), reads=[("x", t)], dma=True)
                    out_done[0] = True

        sq(0)
        sq(1)
        for g in range(4):
            fin(g)
            if g + 2 < 4:
                sq(g + 2)
            upd(g)
        barrier()


    CL = [0]
    out_done = [False]

    def hkeys(r):
        return [("h", k, j) for k in range(KC) for j in range(4 * r, 4 * r + 4)]

    def barrier_keep(m):
        barrier()
        R.release(m)

    def evac_copy(i, out, in_, reads, writes):
        if i % 2 == 0:
            P.op("act", lambda e: e.activation(out=out, in_=in_, func=AF.Copy), reads=reads, writes=writes)
        else:
            P.op("dve", lambda e: e.tensor_copy(out=out, in_=in_), reads=reads, writes=writes)

    def phase_out(w_out_ap, cat_k, extra=None):
        L = CL[0]
        gg_bc = gg_all[:, L]
        wo = R.alloc([128, KC, D], BF16)
        tmp = R.alloc([128, 2, D])
        junk = R.alloc([128, 512], BF16)
        for kh in range(2):
            P.op("pool", lambda e, kh=kh: e.dma_start(out=wo[:, kh * 4:(kh + 1) * 4, :],
                                                       in_=w_out_ap[kh * 512:(kh + 1) * 512, :].rearrange("(k p) d -> p k d", p=128)),
                 writes=[("wo", kh)], dma=True)
        P.op("dve", lambda e: e.memset(stat[:, 0:64], 0.0), writes=["sso"])
        for j in range(NT):
            b0 = 2 * (j % 2)

            def mm(e, j=j, b0=b0):
                for dh in range(2):
                    for k in range(KC):
                        ins = e.matmul(banks[b0 + dh][:], lhsT=cat_k(k)[:, j * 128:(j + 1) * 128], rhs=wo[:, k, dh * 512:(dh + 1) * 512],
                                       start=(k == 0), stop=(k == KC - 1))
                return ins
            P.op("pe", mm, reads=[("wo", 0), ("wo", 1)] + [("cat", k, j // 4) for k in range(KC)], writes=[("b", b0), ("b", b0 + 1)])
            for dh in range(2):
                P.op("act", lambda e, j=j, dh=dh, b0=b0: e.activation(out=junk[:], in_=banks[b0 + dh][:], func=AF.Square,
                                                                       accum_out=stat[:, 32 + 2 * j + dh:33 + 2 * j + dh]),
                     reads=["sso"], writes=[("b", b0 + dh), ("ssq", j, dh), "junk_o"])
            P.op("dve", lambda e, j=j: e.tensor_tensor(out=stat[:, j:j + 1], in0=stat[:, 32 + 2 * j:33 + 2 * j], in1=stat[:, 33 + 2 * j:34 + 2 * j], op=ALU.add),
                 reads=[("ssq", j, 0), ("ssq", j, 1)], writes=[("sso1", j)])
            P.op("act", lambda e, j=j: e.activation(out=stat[:, 16 + j:17 + j], in_=stat[:, j:j + 1], func=AF.Sqrt, bias=NORM_EPS, scale=1.0 / D),
                 reads=[("sso1", j)], writes=[("sso2", j)])
            P.op("dve", lambda e, j=j: e.reciprocal(out=stat[:, 16 + j:17 + j], in_=stat[:, 16 + j:17 + j]), reads=[("sso2", j)], writes=[("rstdo", j)])
            for dh in range(2):
                sl = slice(dh * 512, (dh + 1) * 512)
                P.op("dve", lambda e, j=j, dh=dh, sl=sl, b0=b0: e.scalar_tensor_tensor(
                    out=tmp[:, j % 2, sl], in0=banks[b0 + dh][:], scalar=stat[:, 16 + j:17 + j], in1=gg_bc[:, 0, sl], op0=ALU.mult, op1=ALU.mult),
                    reads=[("rstdo", j), ("gg", L, 0, dh)], writes=[("b", b0 + dh), ("tmpo", j % 2, dh)])
            P.op("dve" if j % 4 else "pool", lambda e, j=j: e.tensor_tensor(out=x_tm[:, j, :], in0=x_tm[:, j, :], in1=tmp[:, j % 2, :], op=ALU.add),
                 reads=[("tmpo", j % 2, 0), ("tmpo", j % 2, 1)], writes=[("x", j)])
            if extra is not None:
                next(extra, None)
        if extra is not None:
            for _ in extra:
                pass
        barrier()

    def phase_mix1():
        cat = R.alloc([128, KC, T], BF16)
        m0 = R.mark()
        AB = R.alloc([128, NT, 4, 256], BF16)
        fT = R.alloc([128, 2, 4, 512], BF16)
        wf = R.alloc([128, KC, 512], BF16)
        cs = R.alloc([128, 256], BF16)
        dblk = [R.alloc([128, 2, 512], BF16) for _ in range(6)]
        P.op("pool", lambda e: e.dma_start(out=wf[:], in_=cd_w_in[:, 1024:1536].rearrange("(k p) f -> p k f", p=128)), writes=["wf"], dma=True)
        P.op("sp", lambda e: e.dma_start(out=cs[:], in_=cs128_d), writes=["cs"], dma=True)
        for r in range(NR):
            rb = r % 2
            for g in range(4):
                bk = g % 2

                def mm(e, r=r, g=g, bk=bk):
                    for k in range(KC):
                        ins = e.matmul(banks[bk][:], lhsT=wf[:, k, g * 128:(g + 1) * 128], rhs=h_cm[:, k, r * 512:(r + 1) * 512],
                                       start=(k == 0), stop=(k == KC - 1))
                    return ins
                P.op("pe", mm, reads=["wf"] + hkeys(r), writes=[("b", bk)])
                evac_copy(g, fT[:, rb, g, :], banks[bk][:], [], [("fT", rb, g), ("b", bk)])
            for jj in range(4):
                j = 4 * r + jj
                b2 = 2 + 2 * (jj % 2)

                def mm2(e, rb=rb, jj=jj, b2=b2):
                    for g in range(4):
                        ins = e.matmul(banks[b2 + g // 2][:, (g % 2) * 256:(g % 2 + 1) * 256], lhsT=fT[:, rb, g, jj * 128:(jj + 1) * 128], rhs=cs[:],
                                       start=True, stop=True)
                    return ins
                P.op("pe", mm2, reads=[("fT", rb, g) for g in range(4)] + ["cs"], writes=[("b", b2), ("b", b2 + 1)])
                for hh in range(2):
                    evac_copy(hh, AB[:, j, 2 * hh:2 * hh + 2, :], banks[b2 + hh][:].rearrange("p (a b) -> p a b", b=256), [],
                              [("AB", j, hh), ("b", b2 + hh)])
        nblk = 0
        for r in range(NR):
            bs = 4 * (r % 2)
            for j in range(NT):
                sl = nblk % 6
                nblk += 1
                P.op("sp", lambda e, r=r, j=j, sl=sl: e.dma_start(out=dblk[sl][:], in_=dft_d[r, j]), writes=[("dblk", sl)], dma=True)

                def mm3(e, j=j, sl=sl, bs=bs):
                    for g in range(4):
                        for c2 in range(2):
                            ins = e.matmul(banks[bs + g][:], lhsT=AB[:, j, g, c2 * 128:(c2 + 1) * 128], rhs=dblk[sl][:, c2, :],
                                           start=(j == 0 and c2 == 0), stop=(j == NT - 1 and c2 == 1))
                    return ins
                P.op("pe", mm3, reads=[("dblk", sl), ("AB", j, 0), ("AB", j, 1)], writes=[("b", bs + g) for g in range(4)])
            for g in range(4):
                evac_copy(g, cat[:, 4 + g, r * 512:(r + 1) * 512], banks[bs + g][:], [], [("cat", 4 + g, r), ("b", bs + g)])
        barrier_keep(m0)
        v_tm = R.alloc([128, NT, 512], BF16)
        wv = R.alloc([128, KC, 512], BF16)
        wu = R.alloc([128, KC, 512], BF16)
        wsr = R.alloc([128, 8, 128])
        wsT = R.alloc([128, 8, 128], BF16)
        bsb = R.alloc([128, 4, 128])
        lnr = R.alloc([128, 2, 512])
        vg = R.alloc([128, 2, 512])
        ug = R.alloc([128, 2, 512])
        tmp2 = R.alloc([128, 2, 512])
        junk = R.alloc([128, 512], BF16)
        st2 = R.alloc([128, NT, 8])
        P.op("pool", lambda e: e.dma_start(out=wv[:], in_=cd_w_in[:, 512:1024].rearrange("(k p) f -> p k f", p=128)), writes=["wv"], dma=True)
        P.op("pool", lambda e: e.dma_start(out=wu[:], in_=cd_w_in[:, 0:512].rearrange("(k p) f -> p k f", p=128)), writes=["wu"], dma=True)
        P.op("sp", lambda e: e.dma_start(out=wsr[:], in_=sg_w_s.rearrange("h p q -> p h q")), writes=["wsr"], dma=True)
        for h in range(8):
            P.op("sp", lambda e, h=h: e.dma_start(out=bsb[(h % 2) * 64:(h % 2 + 1) * 64, h // 2, :], in_=sg_b_s[h:h + 1, :].to_broadcast([64, 128])),
                 writes=[("bsb", h)], dma=True)
        P.op("sp", lambda e: e.dma_start(out=lnr[:, 0, :], in_=sg_ln_g.to_broadcast([128, 512])), writes=[("lnr", 0)], dma=True)
        P.op("sp", lambda e: e.dma_start(out=lnr[:, 1, :], in_=sg_ln_b.to_broadcast([128, 512])), writes=[("lnr", 1)], dma=True)
        for hf in range(2):
            def trw(e, hf=hf):
                for hq in range(4):
                    ins = e.transpose(out=banks[hf][:, hq * 128:(hq + 1) * 128], in_=wsr[:, hf * 4 + hq, :], identity=ident[:])
                return ins
            P.op("pe", trw, reads=["wsr", "ident"], writes=[("b", hf)])
            evac_copy(hf, wsT[:, hf * 4:hf * 4 + 4, :], banks[hf][:].rearrange("p (a b) -> p a b", b=128), [], [("wsT", hf), ("b", hf)])
        P.op("dve", lambda e: e.memset(st2[:], 0.0), writes=["st2"])
        st3 = st2.rearrange("p a b -> p (a b)").rearrange("p (a b) -> p a b", b=NT)
        for j in range(NT):
            vb = 4 + (j % 2)
            jb = j % 2

            def mmv(e, j=j, vb=vb):
                for k in range(KC):
                    ins = e.matmul(banks[vb][:], lhsT=h_cm[:, k, j * 128:(j + 1) * 128], rhs=wv[:, k, :], start=(k == 0), stop=(k == KC - 1))
                return ins
            P.op("pe", mmv, reads=["wv"] + [("h", k, j) for k in range(KC)], writes=[("b", vb)])
            P.op("act", lambda e, j=j, vb=vb, jb=jb: e.activation(out=vg[:, jb, :], in_=banks[vb][:], func=AF.Gelu_apprx_tanh, accum_out=st3[:, 0, j:j + 1]),
                 reads=["st2"], writes=[("vg", jb), ("b", vb), ("st", j, 0)])
            P.op("act", lambda e, j=j, jb=jb: e.activation(out=junk[:], in_=vg[:, jb, :], func=AF.Square, accum_out=st3[:, 1, j:j + 1]),
                 reads=[("vg", jb), "st2"], writes=[("st", j, 1), "junk1"])
            P.op("dve", lambda e, j=j, jb=jb: e.tensor_copy(out=v_tm[:, j, :], in_=vg[:, jb, :]), reads=[("vg", jb)], writes=[("v", j)])
        allst = [("st", j, i) for j in range(NT) for i in range(2)]
        P.op("dve", lambda e: e.tensor_scalar(out=st3[:, 2:4, :], in0=st3[:, 0:2, :], scalar1=1.0 / 512, scalar2=None, op0=ALU.mult), reads=allst, writes=["stm"])
        P.op("dve", lambda e: e.tensor_tensor(out=st3[:, 4, :], in0=st3[:, 2, :], in1=st3[:, 2, :], op=ALU.mult), reads=["stm"], writes=["stq"])
        P.op("dve", lambda e: e.tensor_tensor(out=st3[:, 4, :], in0=st3[:, 4, :], in1=st3[:, 3, :], op=ALU.subtract), reads=["stm", "stq"], writes=["stv"])
        P.op("act", lambda e: e.activation(out=st3[:, 5, :], in_=st3[:, 4, :], func=AF.Sqrt, bias=LN_EPS, scale=-1.0), reads=["stv"], writes=["std"])
        P.op("dve", lambda e: e.reciprocal(out=st3[:, 6, :], in_=st3[:, 5, :]), reads=["std"], writes=["str"])
        P.op("dve", lambda e: e.scalar_tensor_tensor(out=st3[:, 7, :], in0=st3[:, 2, :], scalar=-1.0, in1=st3[:, 6, :], op0=ALU.mult, op1=ALU.mult),
             reads=["stm", "str"], writes=["stb"])
        for r in range(NR):
            for j in range(4 * r, 4 * r + 4):
                jb = j % 2
                P.op("act", lambda e, j=j, jb=jb: e.activation(out=vg[:, jb, :], in_=v_tm[:, j, :], func=AF.Identity, bias=st3[:, 7, j:j + 1], scale=st3[:, 6, j:j + 1]),
                     reads=["stb", "str", ("v", j)], writes=[("vg", jb)])
                P.op("dve", lambda e, jb=jb: e.tensor_tensor(out=vg[:, jb, :], in0=vg[:, jb, :], in1=lnr[:, 0, :], op=ALU.mult),
                     reads=[("lnr", 0)], writes=[("vg", jb)])
                P.op("dve", lambda e, j=j, jb=jb: e.tensor_tensor(out=v_tm[:, j, :], in0=vg[:, jb, :], in1=lnr[:, 1, :], op=ALU.add),
                     reads=[("lnr", 1), ("vg", jb)], writes=[("v", j)])
            for cc in range(4):
                i2 = (r * 4 + cc) % 2

                def mmu(e, cc=cc, r=r, i2=i2):
                    for k in range(KC):
                        ins = e.matmul(banks[i2][:], lhsT=wu[:, k, cc * 128:(cc + 1) * 128], rhs=h_cm[:, k, r * 512:(r + 1) * 512],
                                       start=(k == 0), stop=(k == KC - 1))
                    return ins
                P.op("pe", mmu, reads=["wu"] + hkeys(r), writes=[("b", i2)])
                P.op("act", lambda e, i2=i2: e.activation(out=ug[:, i2, :], in_=banks[i2][:], func=AF.Gelu_apprx_tanh), writes=[("ug", i2), ("b", i2)])

                def mmm(e, cc=cc, r=r, i2=i2):
                    for nl in range(4):
                        for hh in range(2):
                            h = 2 * cc + hh
                            ins = e.matmul(banks[2 + i2][hh * 64:(hh + 1) * 64, nl * 128:(nl + 1) * 128], lhsT=v_tm[:, 4 * r + nl, h * 64:(h + 1) * 64],
                                           rhs=wsT[:, h, :], start=True, stop=True)
                    return ins
                P.op("pe", mmm, reads=[("v", 4 * r + nl) for nl in range(4)] + [("wsT", 0), ("wsT", 1)], writes=[("b", 2 + i2)])
                P.op("dve", lambda e, cc=cc, i2=i2: e.tensor_tensor(
                    out=tmp2[:, i2, :].rearrange("p (a b) -> p a b", b=128), in0=banks[2 + i2][:].rearrange("p (a b) -> p a b", b=128),
                    in1=bsb[:, cc, :].unsqueeze(1).to_broadcast([128, 4, 128]), op=ALU.add),
                    reads=[("bsb", 2 * cc), ("bsb", 2 * cc + 1)], writes=[("tmp2", i2), ("b", 2 + i2)])
                P.op("dve", lambda e, cc=cc, r=r, i2=i2: e.tensor_tensor(out=cat[:, cc, r * 512:(r + 1) * 512], in0=tmp2[:, i2, :], in1=ug[:, i2, :], op=ALU.mult),
                     reads=[("tmp2", i2), ("ug", i2)], writes=[("cat", cc, r)])
        barrier_keep(m0)
        phase_out(cd_w_out, lambda k: cat[:, k, :])

    def phase_mix0():
        catB = R.alloc([128, 4, T], BF16)
        mB = R.mark()
        wB = [R.alloc([128, KC, 256], BF16) for _ in range(2)]
        glu = R.alloc([128, 4, T + 30], BF16)
        dg = R.alloc([128, 4, 31, 128], BF16)
        upre = R.alloc([128, 4, 512])
        ub = R.alloc([128, 4, 512], BF16)
        usq = R.alloc([128, 4, 512], BF16)
        sgt = R.alloc([128, 2, 512])
        t1 = R.alloc([128, 2, 512])
        sbc = R.alloc([128, 2, 512])
        ident_bf = R.alloc([128, 128], BF16)
        ones_bf = R.alloc([128, 128], BF16)
        cw0 = colmap["cv_dw_w"]
        cb0 = colmap["cv_dw_b"]
        lg0 = colmap["cv_ln_g"]
        lb0 = colmap["cv_ln_b"]
        P.op("dve", lambda e: e.tensor_copy(out=ident_bf[:], in_=ident[:]), reads=["ident"], writes=["ident_bf"])
        P.op("dve", lambda e: e.memset(ones_bf[:], 1.0 / 512), writes=["ones_bf"])
        P.op("dve", lambda e: e.memset(glu[:], 0.0), writes=["glu0"])
        for cc in range(4):
            for j in range(31):
                P.op("act" if j % 3 else "dve", lambda e, cc=cc, j=j: (e.activation(out=dg[:, cc, j, :], in_=ident_bf[:], func=AF.Copy, scale=cols[:, cw0 + j * 4 + cc:cw0 + j * 4 + cc + 1])
                                                                       if j % 3 else
                                                                       e.tensor_scalar(out=dg[:, cc, j, :], in0=ident_bf[:], scalar1=cols[:, cw0 + j * 4 + cc:cw0 + j * 4 + cc + 1], scalar2=None, op0=ALU.mult)),
                     reads=["ident_bf", "cols"], writes=[("dg", cc, j)])
        for cc in range(4):
            slot = cc % 2
            P.op("pool", lambda e, cc=cc, slot=slot: e.dma_start(out=wB[slot][:, :, 0:128],
                                                                  in_=ab_w_in[:, 1024 + cc * 128:1024 + (cc + 1) * 128].rearrange("(k p) f -> p k f", p=128)),
                 writes=[("wB", slot, 0)], dma=True)
            P.op("pool", lambda e, cc=cc, slot=slot: e.dma_start(out=wB[slot][:, :, 128:256],
                                                                  in_=ab_w_in[:, 1536 + cc * 128:1536 + (cc + 1) * 128].rearrange("(k p) f -> p k f", p=128)),
                 writes=[("wB", slot, 1)], dma=True)
            for r in range(NR):
                i2 = (cc * NR + r) % 2
                pb = 2 * i2

                def mmg(e, slot=slot, r=r, pb=pb):
                    for k in range(KC):
                        e.matmul(banks[pb][:], lhsT=wB[slot][:, k, 0:128], rhs=h_cm[:, k, r * 512:(r + 1) * 512], start=(k == 0), stop=(k == KC - 1))
                    for k in range(KC):
                        ins = e.matmul(banks[pb + 1][:], lhsT=wB[slot][:, k, 128:256], rhs=h_cm[:, k, r * 512:(r + 1) * 512], start=(k == 0), stop=(k == KC - 1))
                    return ins
                P.op("pe", mmg, reads=[("wB", slot, 0), ("wB", slot, 1)] + hkeys(r), writes=[("b", pb), ("b", pb + 1)])
                P.op("act", lambda e, pb=pb, i2=i2: e.activation(out=sgt[:, i2, :], in_=banks[pb + 1][:], func=AF.Sigmoid), writes=[("sgt", i2), ("b", pb + 1)])
                P.op("dve", lambda e, cc=cc, r=r, pb=pb, i2=i2: e.tensor_tensor(out=glu[:, cc, 15 + r * 512:15 + (r + 1) * 512], in0=banks[pb][:], in1=sgt[:, i2, :], op=ALU.mult),
                     reads=[("sgt", i2), "glu0"], writes=[("glu", cc, r), ("b", pb)])
        for r in range(NR):
            for cc in range(4):
                cb = 4 + (cc % 2)

                def mmc(e, cc=cc, r=r, cb=cb):
                    for j in range(31):
                        ins = e.matmul(banks[cb][:], lhsT=dg[:, cc, j, :], rhs=glu[:, cc, r * 512 + j:r * 512 + j + 512], start=(j == 0), stop=(j == 30))
                    return ins
                P.op("pe", mmc, reads=[("dg", cc, jj) for jj in range(31)] + ["glu0"] + [("glu", cc, rr) for rr in range(max(0, r - 1), min(NR, r + 2))], writes=[("b", cb)])
                P.op("act", lambda e, cc=cc, cb=cb: e.activation(out=upre[:, cc, :], in_=banks[cb][:], func=AF.Identity, bias=cols[:, cb0 + cc:cb0 + cc + 1]),
                     reads=["cols"], writes=[("upre", cc), ("b", cb)])
                P.op("dve", lambda e, cc=cc: e.tensor_copy(out=ub[:, cc, :], in_=upre[:, cc, :]), reads=[("upre", cc)], writes=[("ub", cc)])
                P.op("act", lambda e, cc=cc: e.activation(out=usq[:, cc, :], in_=upre[:, cc, :], func=AF.Square), reads=[("upre", cc)], writes=[("usq", cc)])

            def mms(e):
                for cc in range(4):
                    e.matmul(banks[6][:], lhsT=ones_bf[:], rhs=ub[:, cc, :], start=(cc == 0), stop=(cc == 3))
                for cc in range(4):
                    ins = e.matmul(banks[7][:], lhsT=ones_bf[:], rhs=usq[:, cc, :], start=(cc == 0), stop=(cc == 3))
                return ins
            P.op("pe", mms, reads=["ones_bf"] + [("ub", cc) for cc in range(4)] + [("usq", cc) for cc in range(4)], writes=[("b", 6), ("b", 7)])
            P.op("act", lambda e: e.activation(out=sbc[:, 0, :], in_=banks[6][:], func=AF.Square), writes=[("sbc", 0), ("b", 6)])
            P.op("dve", lambda e: e.tensor_tensor(out=sbc[:, 0, :], in0=sbc[:, 0, :], in1=banks[7][:], op=ALU.subtract), writes=[("sbc", 0), ("b", 7)])
            P.op("act", lambda e: e.activation(out=sbc[:, 1, :], in_=sbc[:, 0, :], func=AF.Sqrt, bias=LN_EPS, scale=-1.0), reads=[("sbc", 0)], writes=[("sbc", 1)])
            P.op("dve", lambda e: e.reciprocal(out=sbc[:, 1, :], in_=sbc[:, 1, :]), writes=[("sbc", 1)])
            for cc in range(4):
                tb = cc % 2
                P.op("dve", lambda e, cc=cc, tb=tb: e.tensor_tensor(out=t1[:, tb, :], in0=upre[:, cc, :], in1=banks[6][:], op=ALU.subtract),
                     reads=[("upre", cc)], writes=[("t1", tb), ("b", 6)])
                P.op("dve", lambda e, tb=tb: e.tensor_tensor(out=t1[:, tb, :], in0=t1[:, tb, :], in1=sbc[:, 1, :], op=ALU.mult),
                     reads=[("sbc", 1)], writes=[("t1", tb)])
                P.op("act", lambda e, cc=cc, r=r, tb=tb: e.activation(out=catB[:, cc, r * 512:(r + 1) * 512], in_=t1[:, tb, :], func=AF.Silu,
                                                                       bias=cols[:, lb0 + cc:lb0 + cc + 1], scale=cols[:, lg0 + cc:lg0 + cc + 1]),
                     reads=[("t1", tb), "cols"], writes=[("cat", 4 + cc, r)])
        barrier_keep(mB)
        catA = R.alloc([128, 4, T], BF16)
        mA = R.mark()
        seg_t = [R.alloc([128, 2, 512]) for _ in range(5)]
        Rt, At, It, Hb, Gt = seg_t
        ct = reg_t[:, mA:mA + 2048].rearrange("p (a b) -> p a b", b=D)
        wA = [R.alloc([128, KC, 256], BF16) for _ in range(2)]
        xpp = R.alloc([128, 2310], BF16)
        XAb = R.alloc([128, 2, CTX + T], BF16)
        Hf = R.alloc([128, T])
        Hc = R.alloc([128, 2, CTX])
        gw = R.alloc([128, 16, 128], BF16)
        dg4 = R.alloc([128, 4, 4, 128], BF16)
        hc_cm = R.alloc([128, KC, CTX], BF16)
        clc = R.alloc([128, 32])
        ncol = R.alloc([128, 16])
        ident_bf = R.alloc([128, 128], BF16)
        junkc = R.alloc([128, D], BF16)
        c4w = colmap["rg_conv_w"]
        c4b = colmap["rg_conv_b"]
        cbr = colmap["rg_b_r"]
        cbi = colmap["rg_b_i"]
        clam = colmap["rg_lambda"]
        ckeys = [("seg", 0, 0), ("seg", 0, 1), ("seg", 1, 0), ("seg", 1, 1)]
        P.op("dve", lambda e: e.memset(stat[:, 0:64], 0.0), writes=["ssc"])
        for jc in range(2):
            P.op("sp", lambda e, jc=jc: e.dma_start(out=ct[:, jc, :], in_=ctx_d[jc * 128:(jc + 1) * 128, :]), writes=ckeys[2 * jc:2 * jc + 2], dma=True)
            P.op("act", lambda e, jc=jc: e.activation(out=junkc[:], in_=ct[:, jc, :], func=AF.Square, accum_out=stat[:, jc:jc + 1]),
                 reads=ckeys[2 * jc:2 * jc + 2] + ["ssc"], writes=[("ssc1", jc), "junkc"])
        P.op("act", lambda e: e.activation(out=stat[:, 16:18], in_=stat[:, 0:2], func=AF.Sqrt, bias=NORM_EPS, scale=1.0 / D),
             reads=[("ssc1", 0), ("ssc1", 1)], writes=["ssc2"])
        P.op("dve", lambda e: e.reciprocal(out=stat[:, 16:18], in_=stat[:, 16:18]), reads=["ssc2"], writes=["rstdc"])
        for jc in range(2):
            P.op("act", lambda e, jc=jc: e.activation(out=ct[:, jc, :], in_=ct[:, jc, :], func=AF.Copy, scale=stat[:, 16 + jc:17 + jc]),
                 reads=["rstdc", ("ssc1", jc)], writes=ckeys[2 * jc:2 * jc + 2])
            b0 = 2 * jc

            def trc(e, jc=jc, b0=b0):
                for k in range(KC):
                    ins = e.transpose(out=banks[b0 + k // 4][:, (k % 4) * 128:(k % 4 + 1) * 128], in_=ct[:, jc, k * 128:(k + 1) * 128], identity=ident[:])
                return ins
            P.op("pe", trc, reads=ckeys[2 * jc:2 * jc + 2] + ["ident"], writes=[("b", b0), ("b", b0 + 1)])
            for k in range(KC):
                src = banks[b0 + k // 4][:, (k % 4) * 128:(k % 4 + 1) * 128]
                if k < 4:
                    P.op("dve", lambda e, k=k, jc=jc, src=src: e.tensor_scalar(out=hc_cm[:, k, jc * 128:(jc + 1) * 128], in0=src, scalar1=modc_all[:, 0, 2, k:k + 1],
                                                                                scalar2=modc_all[:, 0, 3, k:k + 1], op0=ALU.mult, op1=ALU.add),
                         reads=[("modc", 0, 2), ("modc", 0, 3)], writes=[("hc", k, jc), ("b", b0 + k // 4)])
                else:
                    P.op("act", lambda e, k=k, jc=jc, src=src: e.activation(out=hc_cm[:, k, jc * 128:(jc + 1) * 128], in_=src, func=AF.Identity,
                                                                             bias=modc_all[:, 0, 3, k:k + 1], scale=modc_all[:, 0, 2, k:k + 1]),
                         reads=[("modc", 0, 2), ("modc", 0, 3)], writes=[("hc", k, jc), ("b", b0 + k // 4)])
        hckeys = [("hc", k, jc) for k in range(KC) for jc in range(2)]
        P.op("dve", lambda e: e.tensor_copy(out=ident_bf[:], in_=ident[:]), reads=["ident"], writes=["ident_bf"])
        P.op("dve", lambda e: e.memset(gw[:], 0.0), writes=["gw0"])
        P.op("dve", lambda e: e.memset(xpp[:], 0.0), writes=["xpp0"])
        for d in range(2):
            for ri, wsrc in enumerate((rg_w_r, rg_w_i)):
                for hh in range(2):
                    src = wsrc[d].rearrange("(c two) p q -> two p c q", two=2)[hh]
                    P.op("pool", lambda e, d=d, ri=ri, hh=hh, src=src: e.dma_start(
                        out=gw[hh * 64:(hh + 1) * 64, (d * 2 + ri) * 4:(d * 2 + ri) * 4 + 4, hh * 64:(hh + 1) * 64], in_=src),
                        reads=["gw0"], writes=[("gw", d, ri, cc, hh) for cc in range(4)], dma=True)
        for cc in range(4):
            for j in range(4):
                P.op("act", lambda e, cc=cc, j=j: e.activation(out=dg4[:, cc, j, :], in_=ident_bf[:], func=AF.Copy, scale=cols[:, c4w + j * 4 + cc:c4w + j * 4 + cc + 1]),
                     reads=["ident_bf", "cols"], writes=[("dg4", cc, j)])
        P.op("dve", lambda e: e.tensor_scalar(out=ncol[:, 0:8], in0=cols[:, cbr:cbr + 8], scalar1=0.5, scalar2=None, op0=ALU.mult), reads=["cols"], writes=["ncol"])
        P.op("dve", lambda e: e.tensor_scalar(out=ncol[:, 8:16], in0=cols[:, cbi:cbi + 8], scalar1=0.5, scalar2=None, op0=ALU.mult), reads=["cols"], writes=["ncol"])
        P.op("act", lambda e: e.activation(out=clc[:, 16:24], in_=cols[:, clam:clam + 8], func=AF.Exp, scale=-1.0), reads=["cols"], writes=["cl0"])
        P.op("act", lambda e: e.activation(out=clc[:, 24:32], in_=clc[:, 16:24], func=AF.Ln, bias=1.0), reads=["cl0"], writes=["cl1"])
        P.op("dve", lambda e: e.tensor_scalar(out=clc[:, 0:8], in0=clc[:, 24:32], scalar1=-4.0, scalar2=None, op0=ALU.mult), reads=["cl1"], writes=["cl"])
        P.op("dve", lambda e: e.tensor_scalar(out=clc[:, 8:16], in0=clc[:, 24:32], scalar1=-16.0, scalar2=None, op0=ALU.mult), reads=["cl1"], writes=["cl2"])
        segs = [("c", CTX, 0)] + [(r, 512, CTX + r * 512) for r in range(NR)]
        nev = [0]
        for cc in range(4):
            slot = cc % 2
            xb = cc % 2
            P.op("pool", lambda e, cc=cc, slot=slot: e.dma_start(out=wA[slot][:, :, 0:128],
                                                                  in_=ab_w_in[:, cc * 128:(cc + 1) * 128].rearrange("(k p) f -> p k f", p=128)),
                 writes=[("wA", slot, 0)], dma=True)
            P.op("pool", lambda e, cc=cc, slot=slot: e.dma_start(out=wA[slot][:, :, 128:256],
                                                                  in_=ab_w_in[:, 512 + cc * 128:512 + (cc + 1) * 128].rearrange("(k p) f -> p k f", p=128)),
                 writes=[("wA", slot, 1)], dma=True)
            for si, (sid, N, off) in enumerate(segs[1:] + segs[:1]):
                bk = si % 2
                rk = hckeys if sid == "c" else hkeys(sid)
                xo = 1 if sid == "c" else 260 + sid * 512

                def mmx(e, slot=slot, sid=sid, N=N, bk=bk):
                    for k in range(KC):
                        rhs = hc_cm[:, k, :] if sid == "c" else h_cm[:, k, sid * 512:(sid + 1) * 512]
                        ins = e.matmul(banks[bk][:, 0:N], lhsT=wA[slot][:, k, 0:128], rhs=rhs, start=(k == 0), stop=(k == KC - 1))
                    return ins
                P.op("pe", mmx, reads=[("wA", slot, 0)] + rk, writes=[("b", bk)])
                nev[0] += 1
                evac_copy(nev[0], xpp[:, xo:xo + N], banks[bk][:, 0:N], ["xpp0"], [("xpp", sid), ("b", bk)])
            for si, (sid, N, off) in enumerate(segs):
                bk = 2 + si % 2
                base = 0 if sid == "c" else 259 + sid * 512
                nb = [("xpp", "c")] if sid == "c" else [("xpp", rr) for rr in range(max(0, sid - 1), min(NR, sid + 2))]

                def mm4(e, cc=cc, N=N, bk=bk, base=base):
                    for j in range(4):
                        ins = e.matmul(banks[bk][:, 0:N], lhsT=dg4[:, cc, j, :], rhs=xpp[:, base + j:base + j + N], start=(j == 0), stop=(j == 3))
                    return ins
                P.op("pe", mm4, reads=[("dg4", cc, jj) for jj in range(4)] + ["xpp0"] + nb, writes=[("b", bk)])
                P.op("act", lambda e, cc=cc, N=N, off=off, bk=bk, xb=xb: e.activation(out=XAb[:, xb, off:off + N], in_=banks[bk][:, 0:N], func=AF.Identity,
                                                                                       bias=cols[:, c4b + cc:c4b + cc + 1]),
                     reads=["cols"], writes=[("xa", xb, sid), ("b", bk)])
            for d in range(2):
                order = segs if d == 0 else [segs[0]] + segs[:0:-1]
                col = d * 4 + cc
                state = {"carry": None, "ckey": None}

                def seg_front(si, sid, N, off, d=d, cc=cc, col=col, xb=xb):
                    i2 = si % 2
                    bk = 4 + 2 * i2
                    xa = XAb[:, xb, off:off + N]

                    def mmg2(e):
                        e.matmul(banks[bk][:, 0:N], lhsT=gw[:, (d * 2 + 0) * 4 + cc, :], rhs=xa, start=True, stop=True)
                        return e.matmul(banks[bk + 1][:, 0:N], lhsT=gw[:, (d * 2 + 1) * 4 + cc, :], rhs=xa, start=True, stop=True)
                    P.op("pe", mmg2, reads=[("xa", xb, sid)] + [("gw", d, ri, cc, hh) for ri in range(2) for hh in range(2)] + ["gw0"],
                         writes=[("b", bk), ("b", bk + 1)])
                    rr_, ii_, aa_ = Rt[:, i2, 0:N], It[:, i2, 0:N], At[:, i2, 0:N]
                    kR, kI, kA = ("seg", 0, i2), ("seg", 2, i2), ("seg", 1, i2)
                    P.op("act", lambda e: e.activation(out=rr_, in_=banks[bk][:, 0:N], func=AF.Tanh, scale=0.5, bias=ncol[:, col:col + 1]),
                         reads=["ncol"], writes=[kR, ("b", bk)])
                    P.op("act", lambda e: e.activation(out=ii_, in_=banks[bk + 1][:, 0:N], func=AF.Tanh, scale=0.5, bias=ncol[:, 8 + col:9 + col]),
                         reads=["ncol"], writes=[kI, ("b", bk + 1)])
                    P.op("act", lambda e: e.activation(out=aa_, in_=rr_, func=AF.Exp, scale=clc[:, col:col + 1], bias=clc[:, col:col + 1]),
                         reads=[kR, "cl"], writes=[kA])
                    P.op("dve", lambda e: e.tensor_tensor(out=rr_, in0=aa_, in1=aa_, op=ALU.mult), reads=[kA], writes=[kR])
                    P.op("dve", lambda e: e.scalar_tensor_tensor(out=ii_, in0=ii_, scalar=1.0, in1=xa, op0=ALU.add, op1=ALU.mult),
                         reads=[("xa", xb, sid)], writes=[kI])

                def seg_back(si, sid, N, off, d=d, cc=cc, col=col, xb=xb, state=state):
                    i2 = si % 2
                    rr_, ii_, aa_ = Rt[:, i2, 0:N], It[:, i2, 0:N], At[:, i2, 0:N]
                    kR, kI, kA = ("seg", 0, i2), ("seg", 2, i2), ("seg", 1, i2)
                    P.op("act", lambda e: e.activation(out=rr_, in_=rr_, func=AF.Sqrt, bias=1.0, scale=-1.0), writes=[kR])
                    P.op("dve", lambda e: e.scalar_tensor_tensor(out=ii_, in0=ii_, scalar=0.5, in1=rr_, op0=ALU.mult, op1=ALU.mult),
                         reads=[kR], writes=[kI])
                    if d == 0:
                        dst = Hc[:, 0, :] if sid == "c" else Hf[:, sid * 512:(sid + 1) * 512]
                        dkey = ("hcx", 0) if sid == "c" else ("hf", sid)
                        a_ap, b_ap, o_ap = aa_, ii_, dst
                        ncarry = dst[:, N - 1:N]
                    else:
                        dst = Hc[:, 1, :] if sid == "c" else Hb[:, i2, :]
                        dkey = ("hcx", 1) if sid == "c" else ("seg", 3, i2)
                        a_ap, b_ap, o_ap = aa_[:, ::-1], ii_[:, ::-1], dst[:, ::-1]
                        ncarry = dst[:, 0:1]
                    init = 0.0 if state["carry"] is None else state["carry"]
                    ck = [state["ckey"]] if state["ckey"] else []
                    P.op("dve", lambda e: e.tensor_tensor_scan(out=o_ap, data0=a_ap, data1=b_ap, initial=init, op0=ALU.mult, op1=ALU.add),
                         reads=[kA, kI] + ck, writes=[dkey])
                    state["carry"], state["ckey"] = ncarry, dkey
                    if d == 1 and sid != "c":
                        P.op("dve", lambda e: e.tensor_tensor(out=Hf[:, sid * 512:(sid + 1) * 512], in0=Hf[:, sid * 512:(sid + 1) * 512],
                                                               in1=Hb[:, i2, :], op=ALU.add),
                             reads=[("seg", 3, i2)], writes=[("hf", sid)])

                for p0 in range(0, len(order), 2):
                    pair = list(enumerate(order))[p0:p0 + 2]
                    for si, (sid, N, off) in pair:
                        seg_front(si, sid, N, off)
                    for si, (sid, N, off) in pair:
                        seg_back(si, sid, N, off)
            for r in range(NR):
                gb_ = r % 2

                def mmga(e, slot=slot, r=r, gb_=gb_):
                    for k in range(KC):
                        ins = e.matmul(banks[gb_][:], lhsT=wA[slot][:, k, 128:256], rhs=h_cm[:, k, r * 512:(r + 1) * 512],
                                       start=(k == 0), stop=(k == KC - 1))
                    return ins
                P.op("pe", mmga, reads=[("wA", slot, 1)] + hkeys(r), writes=[("b", gb_)])
                P.op("act", lambda e, gb_=gb_: e.activation(out=Gt[:, gb_, :], in_=banks[gb_][:], func=AF.Gelu_apprx_tanh),
                     writes=[("seg", 4, gb_), ("b", gb_)])
                P.op("dve", lambda e, r=r, gb_=gb_, cc=cc: e.tensor_tensor(out=catA[:, cc, r * 512:(r + 1) * 512], in0=Hf[:, r * 512:(r + 1) * 512],
                                                                            in1=Gt[:, gb_, :], op=ALU.mult),
                     reads=[("hf", r), ("seg", 4, gb_)], writes=[("cat", cc, r)])
        barrier_keep(mA)
        phase_out(ab_w_out, lambda k: catA[:, k, :] if k < 4 else catB[:, k - 4, :],
                  extra=(mod_steps(1, False, bb=4, do_barrier=False, nbuf=2, vlist=(0, 1, 2)) if overlap_mod1 else None))

    phase0()
    overlap_mod1 = (tuple(layers) == (0, 1) and "mix" in parts)
    for l in layers:
        CL[0] = l
        if dbg == "p0":
            break
        gen0 = None
        if l == 0 and overlap_mod1:
            gen0 = mod_steps(0, True, bb=5, do_barrier=False, nbuf=3)
            for _ in range(4):
                next(gen0)
        elif not (l == 1 and overlap_mod1):
            phase_mod(l, need_ctx=(l == 0))
        if dbg == "mod":
            break
        if "mix" in parts:
            phase_norm(0, extra=gen0)
            if l == 0:
                phase_mix0()
            else:
                phase_mix1()
            if dbg == "mix":
                break
        if "ffn" in parts:
            phase_norm(1, router=(l == 1),
                       extra=(mod_steps(1, False, bb=5, do_barrier=False, nbuf=3, vlist=(3, 4, 5)) if (l == 0 and overlap_mod1) else None))
            if dbg == "norm":
                break
            if l == 0:
                phase_ffn([(ffn_w1, ffn_w3, ffn_w2, DFF, None)])
            else:
                phase_ffn([(moe_w1[e], moe_w3[e], moe_w2[e], DFFE, e) for e in range(moe_experts)], final=(l == layers[-1]))
    if not out_done[0]:
        for j in range(NT):
            P.op("sp", lambda e, j=j: e.dma_start(out=out_d[j * 128:(j + 1) * 128, :], in_=x_tm[:, j, :]), reads=[("x", j)], dma=True)
    P.emit()
    st.close()
    return nc


def _dft_tables():
    import ml_dtypes
    c = np.arange(128, dtype=np.float64)
    a128 = 2.0 * np.pi * np.outer(c, c) / 128.0
    cs128 = np.concatenate([np.cos(a128), np.sin(a128)], axis=1) / np.sqrt(128.0)
    t = np.arange(T, dtype=np.int64)
    ang = 2.0 * np.pi * ((np.outer(t, t) % T).astype(np.float64)) / T
    CL = np.cos(ang) / np.sqrt(float(T))
    SL = -np.sin(ang) / np.sqrt(float(T))
    both = np.stack([CL, SL], axis=1)
    dft = both.reshape(NT, 128, 2, NR, 512).transpose(3, 0, 1, 2, 4)
    return {"cs128": np.ascontiguousarray(cs128).astype(ml_dtypes.bfloat16),
            "dft": np.ascontiguousarray(dft).astype(ml_dtypes.bfloat16)}


def make_in_maps(inputs):
    f = lambda a: np.ascontiguousarray(np.asarray(a, dtype=np.float32))
    shared = {
        "ident": np.eye(128, dtype=np.float32),
        "ada_w": f(inputs["ada_w"]), "ada_b": f(inputs["ada_b"]), "norm_g": f(inputs["norm_g"]),
        "ffn_w1": f(inputs["ffn_w1"][0]), "ffn_w3": f(inputs["ffn_w3"][0]), "ffn_w2": f(inputs["ffn_w2"][0]),
        "moe_router": f(inputs["moe_router"][0]),
        "moe_w1": f(inputs["moe_w1"][0]), "moe_w3": f(inputs["moe_w3"][0]), "moe_w2": f(inputs["moe_w2"][0]),
        "ab_w_in": f(inputs["ab_w_in"][0]), "rg_conv_w": f(inputs["rg_conv_w"][0]), "rg_conv_b": f(inputs["rg_conv_b"]),
        "rg_w_r": f(inputs["rg_w_r"][0]), "rg_b_r": f(inputs["rg_b_r"][0]), "rg_w_i": f(inputs["rg_w_i"][0]), "rg_b_i": f(inputs["rg_b_i"][0]),
        "rg_lambda": f(inputs["rg_lambda"][0]), "cv_dw_w": f(inputs["cv_dw_w"][0]), "cv_dw_b": f(inputs["cv_dw_b"]),
        "cv_ln_g": f(inputs["cv_ln_g"]), "cv_ln_b": f(inputs["cv_ln_b"]), "ab_w_out": f(inputs["ab_w_out"][0]),
        "cd_w_in": f(inputs["cd_w_in"][0]), "sg_ln_g": f(inputs["sg_ln_g"]), "sg_ln_b": f(inputs["sg_ln_b"]),
        "sg_w_s": f(inputs["sg_w_s"][0]), "sg_b_s": f(inputs["sg_b_s"][0]), "cd_w_out": f(inputs["cd_w_out"][0]),
    }
    shared.update(_dft_tables())
    maps = []
    for b in range(8):
        m = dict(shared)
        m["x"] = f(inputs["x"][b])
        m["ctx"] = f(inputs["ctx"][b])
        m["cc"] = np.concatenate([f(inputs["c"][b]).reshape(8, 128), f(inputs["c_ctx"]).reshape(8, 128)], axis=0)
        maps.append(m)
    return maps


def kernel(**inputs):
    nc = bass.Bass("TRN2", target_bir_lowering=False)
    build(nc)
    maps = make_in_maps(inputs)
    res = run_bass_kernel_spmd(nc, maps, core_ids=list(range(8)))
    return np.stack([np.asarray(r["out"], dtype=np.float32) for r in res.results], axis=0)
```
